# Optimizing a Trainium2 kernel written in Bass

```python
import jax, jax.numpy as jnp
from jax import lax
import numpy as np

D_MODEL = 1024
BATCH = 8
SEQ = 4096
DEPTH = 4

HGRN_HEADS = 4
HGRN_HEAD_DIM = 128
HGRN_WIDTH = HGRN_HEADS * HGRN_HEAD_DIM
HGRN_CHUNK = 64
SB_HEADS = 8
SB_HEAD_DIM = 64
SB_WIDTH = SB_HEADS * SB_HEAD_DIM
SB_BLOCK = 128
FFN_DIM = 3584
N_EXPERTS = 8
TOP_K = 2
RMS_EPS = 1e-6
IN_WIDTH = 4 * HGRN_WIDTH + 3 * SB_WIDTH + 2 * D_MODEL

kernel_name = 'hybrid_hgrn2_stickbreak_moe'


def _split_points():
    sizes = [HGRN_WIDTH] * 4 + [SB_WIDTH] * 3 + [D_MODEL] * 2
    pts, acc = [], 0
    for s in sizes[:-1]:
        acc += s
        pts.append(acc)
    return pts


def rms_norm(x, g):
    x32 = x.astype(jnp.float32)
    y = x32 * lax.rsqrt(jnp.mean(x32 * x32, axis=-1, keepdims=True) + RMS_EPS)
    return (y * g.astype(jnp.float32)).astype(x.dtype)


def hgrn2_chunkwise(q, k, v, log_f):
    B, S, H, Dk = q.shape
    Dv = v.shape[-1]
    n = S // HGRN_CHUNK

    def to_chunks(t):
        return t.reshape(B, n, HGRN_CHUNK, H, t.shape[-1]).transpose(1, 0, 3, 2, 4)

    causal = jnp.tril(jnp.ones((HGRN_CHUNK, HGRN_CHUNK), dtype=bool))[:, :, None]

    def step(s_prev, inp):
        qb, kb, vb, fb = inp
        b = jnp.cumsum(fb, axis=2)
        diff = b[:, :, :, None, :] - b[:, :, None, :, :]
        decay = jnp.exp(jnp.where(causal, diff, -jnp.inf))
        scores = jnp.einsum('bhtd,bhtsd,bhsd->bhts', qb, decay, kb)
        o = (jnp.einsum('bhts,bhsv->bhtv', scores, vb)
             + jnp.einsum('bhtd,bhdv->bhtv', qb * jnp.exp(b), s_prev))
        b_last = b[:, :, -1:, :]
        s_new = (jnp.exp(b_last[:, :, 0, :])[..., None] * s_prev
                 + jnp.einsum('bhsd,bhsv->bhdv', kb * jnp.exp(b_last - b), vb))
        return s_new, o

    s0 = jnp.zeros((B, H, Dk, Dv), jnp.float32)
    _, o = lax.scan(step, s0, (to_chunks(q), to_chunks(k), to_chunks(v), to_chunks(log_f)))
    return o.transpose(1, 0, 3, 2, 4).reshape(B, S, H, Dv)


def stick_breaking_attention(q, k, v):
    B, S, H, d = q.shape
    scale = d ** -0.5
    outs = []
    for blk in range(S // SB_BLOCK):
        t0 = blk * SB_BLOCK
        kl = t0 + SB_BLOCK
        z = jnp.einsum('bthd,bshd->bhts', q[:, t0:kl], k[:, :kl]).astype(jnp.float32) * scale
        t_idx = t0 + jnp.arange(SB_BLOCK)[:, None]
        s_idx = jnp.arange(kl)[None, :]
        strict = s_idx < t_idx
        log_keep = jnp.where(strict, jax.nn.log_sigmoid(-z), 0.0)
        after = lax.cumsum(log_keep, axis=3, reverse=True) - log_keep
        w = jnp.where(strict, jnp.exp(jax.nn.log_sigmoid(z) + after), 0.0)
        outs.append(jnp.einsum('bhts,bshd->bthd', w, v[:, :kl].astype(jnp.float32)))
    return jnp.concatenate(outs, axis=1)


def hybrid_mixer(h, w_in, lb, out_norm, w_branch_a, w_branch_b, w_out):
    B, S, _ = h.shape
    proj = jnp.einsum('bsd,dn->bsn', h, w_in)
    hq, hf, hi, hog, sq, sk, sv, ga, gb = jnp.split(proj, _split_points(), axis=-1)

    def hh(t):
        return t.reshape(B, S, HGRN_HEADS, HGRN_HEAD_DIM).astype(jnp.float32)
    lb = lb.reshape(HGRN_HEADS, HGRN_HEAD_DIM)
    zf = hh(hf)
    log_f = jnp.logaddexp(jnp.log(lb), jnp.log1p(-lb) + jax.nn.log_sigmoid(zf))
    k_in = (1.0 - lb) * jax.nn.sigmoid(-zf)
    o_a = hgrn2_chunkwise(jax.nn.silu(hh(hq)), k_in, hh(hi), log_f)
    o_a = rms_norm(o_a, out_norm) * jax.nn.silu(hh(hog))
    o_a = o_a.reshape(B, S, HGRN_WIDTH).astype(h.dtype)

    def sh(t):
        return t.reshape(B, S, SB_HEADS, SB_HEAD_DIM)
    o_b = stick_breaking_attention(sh(sq), sh(sk), sh(sv)).reshape(B, S, SB_WIDTH).astype(h.dtype)

    y = (jax.nn.sigmoid(ga) * jnp.einsum('bsn,nd->bsd', o_a, w_branch_a)
         + jax.nn.sigmoid(gb) * jnp.einsum('bsn,nd->bsd', o_b, w_branch_b))
    return jnp.einsum('bsd,de->bse', y, w_out)


def swiglu(h, w_gate, w_up, w_down):
    a = jnp.einsum('...d,df->...f', h, w_gate)
    u = jnp.einsum('...d,df->...f', h, w_up)
    return jnp.einsum('...f,fd->...d', jax.nn.silu(a) * u, w_down)


def moe_swiglu(h, w_router, w_gate, w_up, w_down):
    B, S, D = h.shape
    t = h.reshape(B * S, D)
    logits = jnp.einsum('td,de->te', t, w_router).astype(jnp.float32)
    top_val, top_idx = lax.top_k(logits, TOP_K)
    top_w = jax.nn.softmax(top_val, axis=-1)
    combine = jnp.einsum('tk,tke->te', top_w, jax.nn.one_hot(top_idx, N_EXPERTS, dtype=jnp.float32))
    y = jnp.zeros((B * S, D), jnp.float32)
    for e in range(N_EXPERTS):
        y = y + combine[:, e:e + 1] * swiglu(t, w_gate[e], w_up[e], w_down[e]).astype(jnp.float32)
    return y.astype(h.dtype).reshape(B, S, D)


def setup_inputs(seed: int = 0) -> dict:
    key = jax.random.key(seed)
    ks = jax.random.split(key, 20)
    f32 = jnp.float32
    n_dense = (DEPTH + 1) // 2
    n_moe = DEPTH // 2
    res = (2 * DEPTH) ** -0.5

    def nrm(k, shape, fan_in, extra=1.0):
        return jax.random.normal(k, shape, f32) * (fan_in ** -0.5) * extra

    def gain(k, shape):
        return 1.0 + 0.02 * jax.random.normal(k, shape, f32)

    return {
        'x': jax.random.normal(ks[0], (BATCH, SEQ, D_MODEL), f32),
        'mix_norm': gain(ks[1], (DEPTH, D_MODEL)),
        'w_in': nrm(ks[2], (DEPTH, D_MODEL, IN_WIDTH), D_MODEL),
        'hgrn_lb_logits': 0.5 * jax.random.normal(ks[3], (DEPTH, HGRN_WIDTH), f32),
        'hgrn_out_norm': gain(ks[4], (DEPTH, HGRN_HEAD_DIM)),
        'w_branch_hgrn': nrm(ks[5], (DEPTH, HGRN_WIDTH, D_MODEL), HGRN_WIDTH),
        'w_branch_sb': nrm(ks[6], (DEPTH, SB_WIDTH, D_MODEL), SB_WIDTH),
        'w_out': nrm(ks[7], (DEPTH, D_MODEL, D_MODEL), D_MODEL, res),
        'ffn_norm': gain(ks[8], (DEPTH, D_MODEL)),
        'dense_w_gate': nrm(ks[9], (n_dense, D_MODEL, FFN_DIM), D_MODEL),
        'dense_w_up': nrm(ks[10], (n_dense, D_MODEL, FFN_DIM), D_MODEL),
        'dense_w_down': nrm(ks[11], (n_dense, FFN_DIM, D_MODEL), FFN_DIM, res),
        'moe_router': nrm(ks[12], (n_moe, D_MODEL, N_EXPERTS), D_MODEL),
        'moe_w_gate': nrm(ks[13], (n_moe, N_EXPERTS, D_MODEL, FFN_DIM), D_MODEL),
        'moe_w_up': nrm(ks[14], (n_moe, N_EXPERTS, D_MODEL, FFN_DIM), D_MODEL),
        'moe_w_down': nrm(ks[15], (n_moe, N_EXPERTS, FFN_DIM, D_MODEL), FFN_DIM, res),
        'final_norm': gain(ks[16], (D_MODEL,)),
    }


def reference(x, mix_norm, w_in, hgrn_lb_logits, hgrn_out_norm, w_branch_hgrn, w_branch_sb, w_out,
              ffn_norm, dense_w_gate, dense_w_up, dense_w_down, moe_router, moe_w_gate, moe_w_up,
              moe_w_down, final_norm):
    lb_all = jnp.cumsum(jax.nn.softmax(hgrn_lb_logits.astype(jnp.float32), axis=0), axis=0)
    lb_all = lb_all - lb_all[0:1]
    for layer in range(DEPTH):
        h = rms_norm(x, mix_norm[layer])
        x = x + hybrid_mixer(h, w_in[layer], lb_all[layer], hgrn_out_norm[layer],
                             w_branch_hgrn[layer], w_branch_sb[layer], w_out[layer])
        h = rms_norm(x, ffn_norm[layer])
        j = layer // 2
        if layer % 2 == 0:
            x = x + swiglu(h, dense_w_gate[j], dense_w_up[j], dense_w_down[j])
        else:
            x = x + moe_swiglu(h, moe_router[j], moe_w_gate[j], moe_w_up[j], moe_w_down[j])
    return rms_norm(x, final_norm)
```

```python
import numpy as np
from contextlib import ExitStack
import concourse.bass as bass
import concourse.mybir as mybir
from concourse.bass_utils import run_bass_kernel_spmd

F32 = mybir.dt.float32
BF16 = mybir.dt.bfloat16
AF = mybir.ActivationFunctionType
ALU = mybir.AluOpType
AX = mybir.AxisListType

D = 1024
KC = 8
NIN = 5632
HW = 512
FFN = 3584
NE = 8
EPS = 1e-6
CH = 32
ENG = ("pe", "act", "dve", "pool", "sp")


class DSem:
    __slots__ = ("h", "count", "name")

    def __init__(self, name):
        self.h = None
        self.count = 0
        self.name = name


class Prog:
    def __init__(self, nc, st):
        self.nc = nc
        self.st = st
        self.ops = {e: [] for e in ENG}
        self.esem = {e: DSem("e_" + e) for e in ENG}
        for s in self.esem.values():
            s.h = st.enter_context(nc.semaphore(s.name))
        self.dsems = {}
        self.all_ds = []
        self.free_ds = []
        self.writers = {}
        self.readers = {}
        self.known = {e: {} for e in ENG}
        self.nblk = 0

    def _deps(self, eng, reads, writes):
        deps = {}

        def add(d):
            for s, v in d.items():
                if deps.get(s, 0) < v:
                    deps[s] = v
        for b in reads:
            add(self.writers.get(b, {}))
        for b in writes:
            add(self.writers.get(b, {}))
            add(self.readers.get(b, {}))
        waits = []
        kn = self.known[eng]
        for s, v in deps.items():
            if eng == "pe" and s is self.esem["pe"]:
                continue
            if kn.get(s, 0) >= v:
                continue
            kn[s] = v
            waits.append((s, v))
        return waits

    def _commit(self, reads, writes, s, v):
        for b in writes:
            self.writers[b] = {s: v}
            self.readers[b] = {}
        for b in reads:
            r = self.readers.setdefault(b, {})
            if r.get(s, 0) < v:
                r[s] = v

    def op(self, eng, call, reads=(), writes=()):
        m, a, kw = call

        def fn(e, m=m, a=a, kw=kw):
            return getattr(e, m)(*a, **kw)
        waits = self._deps(eng, reads, writes)
        s = self.esem[eng]
        s.count += 1
        self.ops[eng].append((waits, fn, (s, 1)))
        self._commit(reads, writes, s, s.count)

    def dma(self, q, out, in_, reads=(), writes=(), joins=(), key=None, **kw):
        if key is None:
            key = writes[0]
        ds = self.dsems.get(key)
        if ds is None:
            if self.free_ds:
                ds = self.free_ds.pop()
            else:
                ds = DSem("d%d" % len(self.all_ds))
                ds.h = self.st.enter_context(self.nc.semaphore(ds.name))
                self.all_ds.append(ds)
            self.dsems[key] = ds
        waits = self._deps(q, reads, writes)
        ds.count += 16

        def fn(e, out=out, in_=in_, kw=kw):
            return e.dma_start(out=out, in_=in_, **kw)
        self.ops[q].append((waits, fn, (ds, 16)))
        self._commit(reads, writes, ds, ds.count)
        for b in joins:
            w = self.writers.setdefault(b, {})
            w[ds] = ds.count

    def wait_all(self, eng, keys):
        waits = self._deps(eng, keys, ())
        self.ops[eng].append((waits, None, None))

    def flush(self):
        nc = self.nc
        allsems = list(self.esem.values()) + list(self.all_ds)
        for e in ENG:
            waits = []
            kn = self.known[e]
            for s in allsems:
                if s.count > 0 and kn.get(s, 0) < s.count:
                    kn[s] = s.count
                    waits.append((s, s.count))
            self.ops[e].append((waits, None, None))
        ops = self.ops
        self.ops = {e: [] for e in ENG}
        self.writers = {}
        self.readers = {}
        self.free_ds = list(self.all_ds)
        self.dsems = {}
        self.nblk += 1
        with nc.Block() as block:
            def mk(e):
                def body(engobj):
                    for waits, fn, inc in ops[e]:
                        for s, v in waits:
                            engobj.wait_ge(s.h, v)
                        if fn is not None:
                            fn(engobj).then_inc(inc[0].h, inc[1])
                return body
            block.tensor(mk("pe"))
            block.scalar(mk("act"))
            block.vector(mk("dve"))
            block.gpsimd(mk("pool"))
            block.sync(mk("sp"))


def C(m, *a, **kw):
    return (m, a, kw)


def _ring(n):
    i = [0]

    def nxt():
        v = i[0] % n
        i[0] += 1
        return v
    return nxt


class K:
    pass


def build(S=4096, depth=4, debug=False, stop_after=None):
    nc = bass.Bass("TRN2", target_bir_lowering=False)
    NSEG = S // 512
    NT = S // 128
    k = K()
    k.nc = nc
    k.S = S

    def din(name, shape):
        return nc.dram_tensor(name, list(shape), F32, kind="ExternalInput").ap()
    x_in = din("x", (S, D))
    mix_norm = din("mix_norm", (depth, D))
    w_in = din("w_in", (depth, D, NIN))
    lb_logits = din("hgrn_lb_logits", (4, HW))
    out_norm = din("hgrn_out_norm", (depth, 128))
    w_ba = din("w_branch_hgrn", (depth, HW, D))
    w_bb = din("w_branch_sb", (depth, HW, D))
    w_out = din("w_out", (depth, D, D))
    ffn_norm = din("ffn_norm", (depth, D))
    dwg = din("dense_w_gate", (2, D, FFN))
    dwu = din("dense_w_up", (2, D, FFN))
    dwd = din("dense_w_down", (2, FFN, D))
    mrt = din("moe_router", (2, D, NE))
    mwg = din("moe_w_gate", (2, NE, D, FFN))
    mwu = din("moe_w_up", (2, NE, D, FFN))
    mwd = din("moe_w_down", (2, NE, FFN, D))
    final_norm = din("final_norm", (D,))
    out = nc.dram_tensor("out", [S, D], F32, kind="ExternalOutput").ap()

    skind = "ExternalOutput" if debug else "Internal"

    def scr(name, shape, dt):
        return nc.dram_tensor(name, list(shape), dt, kind=skind).ap()
    xres = scr("xres", (S, D), F32)
    hqT = scr("hqT", (HW, S), F32)
    hfT = scr("hfT", (HW, S), F32)
    hi_d = scr("hi_d", (S, HW), BF16)
    hog_d = scr("hog_d", (S, HW), F32)
    sqT = scr("sqT", (HW, S), BF16)
    skT = scr("skT", (HW, S), BF16)
    sv_d = scr("sv_d", (S, HW), BF16)
    gT = scr("gT", (2 * D, S), F32)
    oaT = scr("oaT", (HW, S), BF16)
    obT = scr("obT", (HW, S), BF16)
    comb_d = scr("comb_d", (S, NE), F32)

    with ExitStack() as st:
        p = Prog(nc, st)
        ident = st.enter_context(nc.sbuf_tensor("ident", [128, 128], BF16))
        identf = st.enter_context(nc.sbuf_tensor("identf", [128, 128], F32))
        lbs = st.enter_context(nc.sbuf_tensor("lbs", [128, 4, 4], F32))
        ln1m = st.enter_context(nc.sbuf_tensor("ln1m", [128, 4, 4], F32))
        with ExitStack() as s0:
            lg16 = s0.enter_context(nc.sbuf_tensor("lg16", [16, 128], F32))
            ex = s0.enter_context(nc.sbuf_tensor("ex", [128, 4, 4], F32))
            sm = s0.enter_context(nc.sbuf_tensor("smx", [128, 4], F32))
            pl = s0.enter_context(nc.psum_tensor("pl", [128, 16], F32))
            p.op("pool", C("memset", identf[:], 1.0), writes=["identf"])
            p.op("pool", C("affine_select",
                out=identf[:], in_=identf[:], pattern=[[-1, 128]], compare_op=ALU.is_equal,
                fill=0.0, base=0, channel_multiplier=1), reads=["identf"], writes=["identf"])
            p.op("dve", C("tensor_copy", ident[:], identf[:]), reads=["identf"], writes=["ident"])
            p.dma("sp", lg16[:], lb_logits.rearrange("l (h p) -> (l h) p", p=128), writes=["lg16"])
            p.op("pe", C("transpose", pl[:], lg16[:], identf[0:16, 0:16]), reads=["lg16", "identf"], writes=["pl"])
            p.op("act", C("activation", out=ex[:].rearrange("p l h -> p (l h)"), in_=pl[:], func=AF.Exp),
                 reads=["pl"], writes=["ex"])
            p.op("dve", C("tensor_tensor", out=sm[:], in0=ex[:, 0, :], in1=ex[:, 1, :], op=ALU.add),
                 reads=["ex"], writes=["sm"])
            p.op("dve", C("tensor_tensor", out=sm[:], in0=sm[:], in1=ex[:, 2, :], op=ALU.add),
                 reads=["ex", "sm"], writes=["sm"])
            p.op("dve", C("tensor_tensor", out=sm[:], in0=sm[:], in1=ex[:, 3, :], op=ALU.add),
                 reads=["ex", "sm"], writes=["sm"])
            p.op("dve", C("reciprocal", sm[:], sm[:]), reads=["sm"], writes=["sm"])
            p.op("dve", C("memset", lbs[:, 0, :], 0.0), writes=["lbs0"])
            p.op("dve", C("tensor_tensor", out=lbs[:, 1, :], in0=ex[:, 1, :], in1=sm[:], op=ALU.mult),
                 reads=["ex", "sm"], writes=["lbs1"])
            p.op("dve", C("tensor_tensor", out=ex[:, 2, :], in0=ex[:, 2, :], in1=sm[:], op=ALU.mult),
                 reads=["ex", "sm"], writes=["ex"])
            p.op("dve", C("tensor_tensor", out=ex[:, 3, :], in0=ex[:, 3, :], in1=sm[:], op=ALU.mult),
                 reads=["ex", "sm"], writes=["ex"])
            p.op("dve", C("tensor_tensor", out=lbs[:, 2, :], in0=lbs[:, 1, :], in1=ex[:, 2, :], op=ALU.add),
                 reads=["ex", "lbs1"], writes=["lbs2"])
            p.op("dve", C("tensor_tensor", out=lbs[:, 3, :], in0=lbs[:, 2, :], in1=ex[:, 3, :], op=ALU.add),
                 reads=["ex", "lbs2"], writes=["lbs3"])
            p.op("act", C("activation", out=ln1m[:], in_=lbs[:], func=AF.Ln, scale=-1.0, bias=1.0),
                 reads=["lbs0", "lbs1", "lbs2", "lbs3"], writes=["ln1m"])
            p.flush()
        k.__dict__.update(locals())
        for layer in range(depth):
            xsrc = x_in if layer == 0 else xres
            pass_inproj(k, p, layer, xsrc)
            if stop_after == ("p1", layer):
                break
            if stop_after != ("p2", layer):
                pass_sb(k, p, layer)
            if stop_after == ("p3", layer):
                break
            pass_hgrn(k, p, layer)
            if stop_after == ("p2", layer):
                break
            pass_mixout(k, p, layer, xsrc)
            if layer % 2 == 1:
                pass_router(k, p, layer)
            pass_ffn(k, p, layer, last=(layer == depth - 1))
    return nc


def load_w_bf16(p, dst, src, key, rows=128):
    n = src.shape[-1]
    c0 = 0
    i = 0
    while c0 < n:
        c1 = min(n, c0 + 2048)
        p.dma("pool", dst[:, c0:c1], src[:, c0:c1], writes=[(key, i)])
        c0 = c1
        i += 1
    return [(key, j) for j in range(i)]


def pass_inproj(k, p, layer, xsrc):
    nc = k.nc
    S = k.S
    NSEG = S // 512
    with ExitStack() as st:
        def sb(name, shape, dt):
            return st.enter_context(nc.sbuf_tensor("%s_%d" % (name, p.nblk), list(shape), dt))

        def ps(name, shape, dt):
            return st.enter_context(nc.psum_tensor("%s_%d" % (name, p.nblk), list(shape), dt))
        W = sb("W", (128, KC, NIN), BF16)
        g = sb("g", (128, D), F32)
        xt = [sb("xt%d" % i, (128, 4, D), F32) for i in range(2)]
        junk = sb("junk", (128, D), F32)
        h = sb("h", (128, 4, D), BF16)
        hT = [sb("hT%d" % i, (128, KC, 512), BF16) for i in range(2)]
        ss = sb("ss", (128, 4), F32)
        rstd = sb("rstd", (128, 4), F32)
        sig = [sb("sig%d" % i, (128, 512), F32) for i in range(2)]
        stf = [sb("stf%d" % i, (128, 512), F32) for i in range(4)]
        stb = [sb("stb%d" % i, (128, 512), BF16) for i in range(4)]
        pT = [ps("pT%d" % i, (128, D), BF16) for i in range(2)]
        pm = [ps("pm%d" % i, (128, 512), F32) for i in range(4)]
        pT_r, pm_r, sig_r, stf_r, stb_r = _ring(2), _ring(4), _ring(2), _ring(4), _ring(4)

        wkeys = []
        for kc in range(KC):
            wkeys.append(load_w_bf16(p, W[:, kc, :], k.w_in[layer, kc * 128:(kc + 1) * 128, :], ("W", kc)))
        p.dma("sp", g[:], k.mix_norm[layer, :].partition_broadcast(128), writes=["g"])

        def load_x(seg):
            b = seg % 2
            p.dma("sp", xt[b][:], xsrc[seg * 512:(seg + 1) * 512, :].rearrange("(j p) d -> p j d", p=128),
                  writes=[("xt", b)])
        load_x(0)
        for seg in range(NSEG):
            b = seg % 2
            if seg + 1 < NSEG:
                load_x(seg + 1)
            for j in range(4):
                p.op("dve", C("scalar_tensor_tensor",
                    out=junk[:], in0=xt[b][:, j, :], scalar=1.0, in1=xt[b][:, j, :],
                    op0=ALU.mult, op1=ALU.mult, accum_out=ss[:, j:j + 1]),
                    reads=[("xt", b)], writes=["junk", ("ss", j)])
            sskeys = [("ss", j) for j in range(4)]
            p.op("dve", C("tensor_scalar", out=rstd[:], in0=ss[:], scalar1=1.0 / D, scalar2=EPS,
                                                   op0=ALU.mult, op1=ALU.add), reads=sskeys, writes=["rstd"])
            p.op("act", C("activation", out=rstd[:], in_=rstd[:], func=AF.Sqrt), reads=["rstd"], writes=["rstd"])
            p.op("dve", C("reciprocal", rstd[:], rstd[:]), reads=["rstd"], writes=["rstd"])
            for j in range(4):
                p.op("dve", C("scalar_tensor_tensor",
                    out=h[:, j, :], in0=xt[b][:, j, :], scalar=rstd[:, j:j + 1], in1=g[:],
                    op0=ALU.mult, op1=ALU.mult), reads=[("xt", b), "rstd", "g"], writes=[("h", j)])
            for j in range(4):
                tb = pT_r()
                for kc in range(KC):
                    p.op("pe", C("transpose",
                        pT[tb][:, kc * 128:(kc + 1) * 128], h[:, j, kc * 128:(kc + 1) * 128], k.ident[:]),
                        reads=[("h", j), "ident"], writes=[("pT", tb)])
                p.op("dve", C("tensor_copy",
                    hT[b][:, :, j * 128:(j + 1) * 128], pT[tb][:].rearrange("p (c t) -> p c t", c=KC)),
                    reads=[("pT", tb)], writes=[("hT", b, j)])
            hTk = [("hT", b, j) for j in range(4)]
            def fm_chunk(col0):
                pb = pm_r()
                for kc in range(KC):
                    wk = [kk for kk in wkeys[kc]]
                    p.op("pe", C("matmul",
                        pm[pb][:], W[:, kc, col0:col0 + 128], hT[b][:, kc, :], start=(kc == 0), stop=(kc == KC - 1)),
                        reads=hTk + wk, writes=[("pm", pb)])
                return pb
            tsl = slice(seg * 512, (seg + 1) * 512)
            for c in range(4):
                pb = fm_chunk(0 + c * 128)
                sb_ = sig_r()
                fb = stf_r()
                p.op("act", C("activation", out=sig[sb_][:], in_=pm[pb][:], func=AF.Sigmoid),
                     reads=[("pm", pb)], writes=[("sig", sb_)])
                p.op("dve", C("tensor_tensor", out=stf[fb][:], in0=pm[pb][:], in1=sig[sb_][:], op=ALU.mult),
                     reads=[("pm", pb), ("sig", sb_)], writes=[("stf", fb)])
                p.dma("sp", k.hqT[c * 128:(c + 1) * 128, tsl], stf[fb][:], reads=[("stf", fb)], joins=["hqT"], key=("o", "stf", fb))
            for c in range(4):
                pb = fm_chunk(512 + c * 128)
                fb = stf_r()
                p.op("act", C("activation", out=stf[fb][:], in_=pm[pb][:], func=AF.Copy),
                     reads=[("pm", pb)], writes=[("stf", fb)])
                p.dma("sp", k.hfT[c * 128:(c + 1) * 128, tsl], stf[fb][:], reads=[("stf", fb)], joins=["hfT"], key=("o", "stf", fb))
            for c in range(8):
                pb = fm_chunk(2048 + c * 128 if c < 4 else 2560 + (c - 4) * 128)
                bb = stb_r()
                sc = 0.125 if c < 4 else 1.0
                p.op("dve", C("tensor_scalar", out=stb[bb][:], in0=pm[pb][:], scalar1=sc, scalar2=None, op0=ALU.mult),
                     reads=[("pm", pb)], writes=[("stb", bb)])
                dst = k.sqT if c < 4 else k.skT
                cc = c % 4
                p.dma("sp", dst[cc * 128:(cc + 1) * 128, tsl], stb[bb][:], reads=[("stb", bb)],
                      joins=["sqT" if c < 4 else "skT"], key=("o", "stb", bb))
            for c in range(16):
                pb = fm_chunk(3584 + c * 128)
                fb = stf_r()
                p.op("act", C("activation", out=stf[fb][:], in_=pm[pb][:], func=AF.Sigmoid),
                     reads=[("pm", pb)], writes=[("stf", fb)])
                p.dma("sp", k.gT[c * 128:(c + 1) * 128, tsl], stf[fb][:], reads=[("stf", fb)], joins=["gT"], key=("o", "stf", fb))
            for j in range(4):
                rsl = slice(seg * 512 + j * 128, seg * 512 + (j + 1) * 128)
                for which, col0 in (("hi", 1024), ("hog", 1536), ("sv", 3072)):
                    pb = pm_r()
                    for kc in range(KC):
                        p.op("pe", C("matmul",
                            pm[pb][:], hT[b][:, kc, j * 128:(j + 1) * 128], W[:, kc, col0:col0 + 512],
                            start=(kc == 0), stop=(kc == KC - 1)),
                            reads=[("hT", b, j)] + wkeys[kc], writes=[("pm", pb)])
                    if which == "hog":
                        sb_ = sig_r()
                        fb = stf_r()
                        p.op("act", C("activation", out=sig[sb_][:], in_=pm[pb][:], func=AF.Sigmoid),
                             reads=[("pm", pb)], writes=[("sig", sb_)])
                        p.op("dve", C("tensor_tensor", out=stf[fb][:], in0=pm[pb][:], in1=sig[sb_][:], op=ALU.mult),
                             reads=[("pm", pb), ("sig", sb_)], writes=[("stf", fb)])
                        p.dma("sp", k.hog_d[rsl, :], stf[fb][:], reads=[("stf", fb)], joins=["hog_d"], key=("o", "stf", fb))
                    else:
                        bb = stb_r()
                        p.op("act", C("activation", out=stb[bb][:], in_=pm[pb][:], func=AF.Copy),
                             reads=[("pm", pb)], writes=[("stb", bb)])
                        dst = k.hi_d if which == "hi" else k.sv_d
                        p.dma("sp", dst[rsl, :], stb[bb][:], reads=[("stb", bb)],
                              joins=["hi_d" if which == "hi" else "sv_d"], key=("o", "stb", bb))
        p.flush()


def pass_sb(k, p, layer):
    nc = k.nc
    S = k.S
    NT = S // 128
    NG = NT // 4
    LA = 3
    with ExitStack() as st:
        def sb(name, shape, dt):
            return st.enter_context(nc.sbuf_tensor("%s_%d" % (name, p.nblk), list(shape), dt))

        def ps(name, shape, dt):
            return st.enter_context(nc.psum_tensor("%s_%d" % (name, p.nblk), list(shape), dt))
        kT = sb("kT", (128, 4, S), BF16)
        qT = [sb("qT%d" % i, (128, 8, 512), BF16) for i in range(2)]
        v = sb("v", (128, NT, 512), BF16)
        msk = sb("msk", (128, 4, 4, 128), F32)
        ntri = sb("ntri", (128, 128), BF16)
        nones = sb("nones", (128, 128), BF16)
        tmpf = sb("tmpf", (128, 128), F32)
        NE_, NSP, NW, NC16 = 4, 6, 4, 5
        E = [sb("E%d" % i, (128, 512), F32) for i in range(NE_)]
        SPf = [sb("SPf%d" % i, (128, 512), F32) for i in range(2)]
        Wf = [sb("Wf%d" % i, (128, 512), F32) for i in range(2)]
        SP = [sb("SP%d" % i, (128, 512), BF16) for i in range(NSP)]
        Wt = [sb("Wt%d" % i, (128, 512), BF16) for i in range(NW)]
        C32 = [sb("C32%d" % i, (128, 512), F32) for i in range(2)]
        C16 = [sb("C16%d" % i, (128, 512), BF16) for i in range(NC16)]
        obt = [sb("obt%d" % i, (128, 512), BF16) for i in range(2)]
        zA = [ps("zA%d" % i, (128, 512), F32) for i in range(3)]
        zB = [ps("zB%d" % i, (128, 512), F32) for i in range(2)]
        po = [ps("po%d" % i, (128, 512), F32) for i in range(2)]
        E_r, SP_r, W_r, C16_r, zA_r, zB_r, obt_r, spf_r, wf_r = (_ring(NE_), _ring(NSP), _ring(NW), _ring(NC16), _ring(3),
                                                                 _ring(2), _ring(2), _ring(2), _ring(2))
        p.op("pool", C("memset", msk[:], 1.0), writes=["msk"])
        for r in range(4):
            p.op("pool", C("affine_select", out=msk[:, r, :, :], in_=msk[:, r, :, :], pattern=[[128, 4], [1, 128]],
                           compare_op=ALU.is_gt, fill=0.0, base=-128 * r, channel_multiplier=-1),
                 reads=["msk"], writes=["msk"])
        p.op("pool", C("memset", tmpf[:], -1.0), writes=["tmpf"])
        p.op("dve", C("tensor_copy", nones[:], tmpf[:]), reads=["tmpf"], writes=["nones"])
        p.op("pool", C("affine_select", out=tmpf[:], in_=tmpf[:], pattern=[[-1, 128]],
                       compare_op=ALU.is_ge, fill=0.0, base=0, channel_multiplier=1), reads=["tmpf", "nones"], writes=["tmpf"])
        p.op("dve", C("tensor_copy", ntri[:], tmpf[:]), reads=["tmpf"], writes=["ntri"])
        for c in range(4):
            p.dma("sp", kT[:, c, :], k.skT[c * 128:(c + 1) * 128, :], writes=[("kT", c)])
        for i in range(2):
            p.op("pool", C("memset", qT[i][:], 0.0), writes=[("qT", i, 0), ("qT", i, 1)])
        for j0 in range(0, NT, 8):
            j1 = min(NT, j0 + 8)
            p.dma("sp", v[:, j0:j1, :], k.sv_d[j0 * 128:j1 * 128, :].rearrange("(j p) n -> p j n", p=128),
                  writes=[("v", j0 // 8)])
        steps = [(m, hd, kb) for m in range(NG) for hd in range(8) for kb in range(4 * m + 3, -1, -1)]
        stt = {}
        cst = {"c32": 0}

        def stage_a(i):
            m, hd, kb = steps[i]
            top = (kb == 4 * m + 3)
            r = kb - 4 * m
            qi = m % 2
            if top and hd == 0:
                qsrc = k.sqT[:, m * 512:(m + 1) * 512].rearrange("(c two d) t -> two d c t", two=2, d=64)
                for half in range(2):
                    p.dma("sp", qT[qi][half * 64:(half + 1) * 64, half::2, :], qsrc[half], writes=[("qT", qi, half)])
            a = zA_r()
            p.op("pe", C("matmul", zA[a][:], kT[:, hd // 2, kb * 128:(kb + 1) * 128], qT[qi][:, hd, :], start=True, stop=True),
                 reads=[("kT", hd // 2), ("qT", qi, hd % 2)], writes=[("zA", a)])
            ei = E_r()
            si = SP_r()
            p.op("act", C("activation", out=E[ei][:], in_=zA[a][:], func=AF.Exp), reads=[("zA", a)], writes=[("E", ei)])
            if r >= 0:
                fi = spf_r()
                p.op("act", C("activation", out=SPf[fi][:], in_=E[ei][:], func=AF.Ln, bias=1.0),
                     reads=[("E", ei)], writes=[("SPf", fi)])
                p.op("dve", C("tensor_tensor", out=SP[si][:], in0=SPf[fi][:],
                              in1=msk[:, r, :, :].rearrange("p q t -> p (q t)"), op=ALU.mult),
                     reads=[("SPf", fi), "msk"], writes=[("SP", si)])
            else:
                p.op("act", C("activation", out=SP[si][:], in_=E[ei][:], func=AF.Ln, bias=1.0),
                     reads=[("E", ei)], writes=[("SP", si)])
            c_use = None if top else cst["c16"]
            if kb > 0:
                ci = C16_r()
                if top:
                    cn = cst["c32"] = 1 - cst["c32"]
                    p.op("dve", C("tensor_copy", C32[cn][:], SP[si][:]), reads=[("SP", si)], writes=[("C32", cn)])
                    p.op("dve", C("tensor_copy", C16[ci][:], SP[si][:]), reads=[("SP", si)], writes=[("C16", ci)])
                else:
                    co = cst["c32"]
                    cn = cst["c32"] = 1 - co
                    p.op("dve", C("tensor_tensor", out=C16[ci][:], in0=C32[co][:], in1=SP[si][:], op=ALU.add),
                         reads=[("SP", si), ("C32", co)], writes=[("C16", ci)])
                    p.op("dve", C("tensor_tensor", out=C32[cn][:], in0=C32[co][:], in1=SP[si][:], op=ALU.add),
                         reads=[("SP", si), ("C32", co)], writes=[("C32", cn)])
                cst["c16"] = ci
            stt[i] = (si, c_use, qi)

        def stage_b(i):
            m, hd, kb = steps[i]
            top = (kb == 4 * m + 3)
            r = kb - 4 * m
            si, c_use, qi = stt.pop(i)
            pob = hd % 2
            bz = zB_r()
            p.op("pe", C("matmul", zB[bz][:], kT[:, hd // 2, kb * 128:(kb + 1) * 128], qT[qi][:, hd, :], start=True, stop=False,
                         skip_group_check=True), reads=[("kT", hd // 2), ("qT", qi, hd % 2)], writes=[("zB", bz)])
            p.op("pe", C("matmul", zB[bz][:], ntri[:], SP[si][:], start=False, stop=top, skip_group_check=True),
                 reads=["ntri", ("SP", si)], writes=[("zB", bz)])
            if not top:
                p.op("pe", C("matmul", zB[bz][:], nones[:], C16[c_use][:], start=False, stop=True, skip_group_check=True),
                     reads=["nones", ("C16", c_use)], writes=[("zB", bz)])
            wi = W_r()
            if r >= 0:
                fi = wf_r()
                p.op("act", C("activation", out=Wf[fi][:], in_=zB[bz][:], func=AF.Exp), reads=[("zB", bz)], writes=[("Wf", fi)])
                p.op("dve", C("tensor_tensor", out=Wt[wi][:], in0=Wf[fi][:],
                              in1=msk[:, r, :, :].rearrange("p q t -> p (q t)"), op=ALU.mult),
                     reads=[("Wf", fi), "msk"], writes=[("W", wi)])
            else:
                p.op("act", C("activation", out=Wt[wi][:], in_=zB[bz][:], func=AF.Exp), reads=[("zB", bz)], writes=[("W", wi)])
            stt[("b", i)] = wi

        def stage_b2(i):
            m, hd, kb = steps[i]
            top = (kb == 4 * m + 3)
            pob = hd % 2
            wi = stt.pop(("b", i))
            pc = (hd // 2) * 128
            p.op("pe", C("matmul", po[pob][:], v[:, kb, pc:pc + 128], Wt[wi][:], start=top, stop=(kb == 0),
                         skip_group_check=True), reads=[("v", kb // 8), ("W", wi)], writes=[("po", pob)])
            if kb == 0:
                oi = obt_r()
                hr = slice((hd % 2) * 64, (hd % 2) * 64 + 64)
                p.op("dve", C("tensor_copy", obt[oi][hr, :], po[pob][hr, :]), reads=[("po", pob)], writes=[("obt", oi)])
                p.dma("sp", k.obT[hd * 64:(hd + 1) * 64, m * 512:(m + 1) * 512], obt[oi][hr, :], reads=[("obt", oi)],
                      joins=["obT"], key=("o", "obt", oi))
        n = len(steps)
        for i in range(min(LA, n)):
            stage_a(i)
        for i in range(n):
            if i + LA < n:
                stage_a(i + LA)
            stage_b(i)
            if i >= 1:
                stage_b2(i - 1)
        stage_b2(n - 1)
        p.flush()


def pass_hgrn(k, p, layer):
    nc = k.nc
    S = k.S
    T = 256
    NC_ = T // CH
    NSEG = S // T
    with ExitStack() as st:
        def sb(name, shape, dt):
            return st.enter_context(nc.sbuf_tensor("%s_%d" % (name, p.nblk), list(shape), dt))

        def ps(name, shape, dt):
            return st.enter_context(nc.psum_tensor("%s_%d" % (name, p.nblk), list(shape), dt))
        ZF2 = [sb("ZF%d" % i, (128, 4, T), F32) for i in range(2)]
        Q2 = [sb("Q%d" % i, (128, 4, T), F32) for i in range(2)]
        V2 = [sb("V%d" % i, (32, NC_, 512), BF16) for i in range(2)]
        HOG2 = [sb("HOG%d" % i, (32, NC_, 512), F32) for i in range(2)]
        EB2 = [sb("EB%d" % i, (128, 4, T), F32) for i in range(2)]
        QE2 = [sb("QE%d" % i, (128, 4, T), BF16) for i in range(2)]
        KE2 = [sb("KE%d" % i, (128, 4, T), BF16) for i in range(2)]
        KL2 = [sb("KL%d" % i, (128, 4, T), BF16) for i in range(2)]
        tn = ("E", "L1", "L2", "LF", "Bc", "T1", "KK", "ENB", "KE32")
        tmp = {n: [sb("%s%d" % (n, i), (128, T), F32) for i in range(2)] for n in tn}
        rmask = sb("rmask", (128, T), F32)
        cmask = sb("cmask", (32, 4, 32), F32)
        GN = sb("GN", (32, 128), F32)
        S32 = sb("S32", (128, 4, 128), F32)
        Sbf = sb("Sbf", (128, 4, 128), BF16)
        SCM = [sb("SCM%d" % i, (32, 4, 32), BF16) for i in range(2)]
        KLT = [sb("KLT%d" % i, (32, 4, 128), BF16) for i in range(2)]
        OS2 = [sb("OS%d" % i, (32, NC_, 512), F32) for i in range(2)]
        SQ = sb("SQ", (32, NC_, 512), F32)
        OG = sb("OG", (32, NC_, 512), BF16)
        ssum = sb("ssum", (32, NC_ * 4), F32)
        OAT = sb("OAT", (128, 4, T), BF16)
        psc = [ps("psc%d" % i, (32, 4, 32), F32) for i in range(2)]
        pkl = [ps("pkl%d" % i, (32, 4, 128), BF16) for i in range(2)]
        pso = ps("pso", (32, 512), F32)
        pkv = [ps("pkv%d" % i, (128, 4, 128), F32) for i in range(2)]
        pot1 = ps("pot", (128, 4, T), BF16)
        pot = [pot1[:, 0:2, :], pot1[:, 2:4, :]]
        p.op("pool", C("memset", rmask[:], 1.0), writes=["rmask"])
        p.op("pool", C("memset", rmask[:].rearrange("p (c t) -> p c t", t=CH)[:, :, 0:1], 0.0), reads=["rmask"], writes=["rmask"])
        p.op("pool", C("memset", cmask[:], 1.0), writes=["cmask"])
        p.op("pool", C("affine_select", out=cmask[:], in_=cmask[:], pattern=[[0, 4], [1, 32]], compare_op=ALU.is_ge,
                       fill=0.0, base=0, channel_multiplier=-1), reads=["cmask"], writes=["cmask"])
        p.op("dve", C("memset", S32[:], 0.0), writes=[("S32", h) for h in range(4)])
        p.op("dve", C("memset", Sbf[:], 0.0), writes=[("Sbf", h) for h in range(4)])
        p.dma("sp", GN[:], k.out_norm[layer, :].partition_broadcast(32), writes=["GN"])
        lbv = k.lbs
        l1v = k.ln1m
        tr = _ring(2)
        def loads(seg):
            sb_ = seg % 2
            tsl = slice(seg * T, (seg + 1) * T)
            p.dma("sp", ZF2[sb_][:], k.hfT[:, tsl].rearrange("(h p) t -> p h t", p=128), writes=[("ZF", sb_)])
            p.dma("sp", Q2[sb_][:], k.hqT[:, tsl].rearrange("(h p) t -> p h t", p=128), writes=[("Q", sb_)])
            p.dma("sp", V2[sb_][:], k.hi_d[tsl, :].rearrange("(c p) n -> p c n", p=CH), writes=[("V", sb_)])
            if seg == 0:
                load_hog(seg)

        def load_hog(seg):
            sb_ = seg % 2
            tsl = slice(seg * T, (seg + 1) * T)
            p.dma("sp", HOG2[sb_][:], k.hog_d[tsl, :].rearrange("(c p) n -> p c n", p=CH), writes=[("HOG", sb_)])

        def ew_head(seg, h):
            sb_ = seg % 2
            ZF, Q, EB, QE, KE, KL = ZF2[sb_], Q2[sb_], EB2[sb_], QE2[sb_], KE2[sb_], KL2[sb_]
            i = tr()
            t_ = {n: tmp[n][i] for n in tn}
            K_ = {n: (n, i) for n in tn}
            lb_ap = lbv[:, layer, h:h + 1]
            l1m_ap = l1v[:, layer, h:h + 1]
            zk, qk_ = ("ZF", sb_), ("Q", sb_)
            ebk, qek, kek, klk = ("EB", sb_, h), ("QE", sb_, h), ("KE", sb_, h), ("KL", sb_, h)
            p.op("act", C("activation", out=t_["E"][:], in_=ZF[:, h, :], func=AF.Exp, scale=-1.0), reads=[zk], writes=[K_["E"]])
            p.op("act", C("activation", out=t_["L1"][:], in_=t_["E"][:], func=AF.Ln, bias=1.0), reads=[K_["E"]], writes=[K_["L1"]])
            p.op("act", C("activation", out=t_["L2"][:], in_=t_["E"][:], func=AF.Ln, scale=lb_ap, bias=1.0),
                 reads=[K_["E"]], writes=[K_["L2"]])
            p.op("dve", C("tensor_tensor", out=t_["LF"][:], in0=t_["L2"][:], in1=t_["L1"][:], op=ALU.subtract),
                 reads=[K_["L1"], K_["L2"]], writes=[K_["LF"]])
            p.op("dve", C("tensor_tensor_scan", out=t_["Bc"][:], data0=rmask[:], data1=t_["LF"][:], initial=0.0,
                          op0=ALU.mult, op1=ALU.add), reads=["rmask", K_["LF"]], writes=[K_["Bc"]])
            p.op("dve", C("tensor_tensor", out=t_["T1"][:], in0=ZF[:, h, :], in1=t_["L1"][:], op=ALU.add),
                 reads=[zk, K_["L1"]], writes=[K_["T1"]])
            p.op("act", C("activation", out=t_["KK"][:], in_=t_["T1"][:], func=AF.Exp, scale=-1.0, bias=l1m_ap),
                 reads=[K_["T1"]], writes=[K_["KK"]])
            p.op("act", C("activation", out=EB[:, h, :], in_=t_["Bc"][:], func=AF.Exp), reads=[K_["Bc"]], writes=[ebk])
            p.op("act", C("activation", out=t_["ENB"][:], in_=t_["Bc"][:], func=AF.Exp, scale=-1.0),
                 reads=[K_["Bc"]], writes=[K_["ENB"]])
            p.op("dve", C("tensor_tensor", out=QE[:, h, :], in0=Q[:, h, :], in1=EB[:, h, :], op=ALU.mult),
                 reads=[qk_, ebk], writes=[qek])
            p.op("dve", C("tensor_tensor", out=t_["KE32"][:], in0=t_["KK"][:], in1=t_["ENB"][:], op=ALU.mult),
                 reads=[K_["KK"], K_["ENB"]], writes=[K_["KE32"]])
            p.op("act", C("activation", out=KE[:, h, :], in_=t_["KE32"][:], func=AF.Copy), reads=[K_["KE32"]], writes=[kek])
            p.op("dve", C("tensor_tensor", out=KL[:, h, :].rearrange("p (c t) -> p c t", t=CH),
                          in0=t_["KE32"][:].rearrange("p (c t) -> p c t", t=CH),
                          in1=EB[:, h, :].rearrange("p (c t) -> p c t", t=CH)[:, :, CH - 1:CH].broadcast_to([128, NC_, CH]),
                          op=ALU.mult), reads=[K_["KE32"], ebk], writes=[klk])
        def post_part(seg, part):
            sb_ = seg % 2
            tsl = slice(seg * T, (seg + 1) * T)
            OS, HOG = OS2[sb_], HOG2[sb_]
            osk = [("OS", sb_, c) for c in range(NC_)]
            NG = NC_ * 4
            osv = OS[:].rearrange("p c (h v) -> p (c h) v", v=128)
            if part == 0:
                p.op("act", C("activation", out=SQ[:], in_=OS[:], func=AF.Square), reads=osk, writes=["SQ"])
                p.op("dve", C("tensor_reduce", out=ssum[:], in_=SQ[:].rearrange("p c (h v) -> p (c h) v", v=128), axis=AX.X, op=ALU.add),
                     reads=["SQ"], writes=["ssum"])
                p.op("act", C("activation", out=ssum[:], in_=ssum[:], func=AF.Ln, scale=1.0 / 128, bias=EPS), reads=["ssum"], writes=["ssum"])
                p.op("act", C("activation", out=ssum[:], in_=ssum[:], func=AF.Exp, scale=-0.5), reads=["ssum"], writes=["ssum"])
            elif part == 1:
                p.op("dve", C("tensor_tensor", out=osv, in0=osv, in1=ssum[:].unsqueeze(2).broadcast_to([32, NG, 128]), op=ALU.mult),
                     reads=osk + ["ssum"], writes=[("OSn", sb_)])
                hgv = HOG[:].rearrange("p c (h v) -> p (c h) v", v=128)
                p.op("dve", C("tensor_tensor", out=hgv, in0=hgv, in1=GN[:].unsqueeze(1).broadcast_to([32, NG, 128]), op=ALU.mult),
                     reads=[("HOG", sb_), "GN"], writes=[("HOG", sb_)])
            else:
                if part == 2:
                    p.op("dve", C("tensor_tensor", out=OG[:], in0=OS[:], in1=HOG[:], op=ALU.mult),
                         reads=[("OSn", sb_), ("HOG", sb_)], writes=["OG"])
                hp = part - 2
                for h2 in range(2):
                    h = hp * 2 + h2
                    for c in range(NC_):
                        p.op("pe", C("transpose", pot[hp][:, h2, c * CH:(c + 1) * CH], OG[:, c, h * 128:(h + 1) * 128],
                                     k.ident[0:32, 0:32]), reads=["OG", "ident"], writes=[("pot", 0)])
                p.op("dve", C("tensor_copy", OAT[:, hp * 2:hp * 2 + 2, :], pot[hp]), reads=[("pot", 0)], writes=[("OAT", hp)])
                if part == 3:
                    p.dma("sp", k.oaT[:, tsl].rearrange("(h p) t -> p h t", p=128), OAT[:], reads=[("OAT", 0), ("OAT", 1)],
                          joins=["oaT"], key=("o", "OAT"))
        loads(0)
        for h in range(4):
            ew_head(0, h)
        for seg in range(NSEG):
            sb_ = seg % 2
            tsl = slice(seg * T, (seg + 1) * T)
            ZF, Q, V, HOG = ZF2[sb_], Q2[sb_], V2[sb_], HOG2[sb_]
            EB, QE, KE, KL = EB2[sb_], QE2[sb_], KE2[sb_], KL2[sb_]
            if seg + 1 < NSEG:
                loads(seg + 1)
            def chunk_a(c):
                cs = slice(c * CH, (c + 1) * CH)
                ci = c % 2
                for h in range(4):
                    p.op("pe", C("matmul", psc[ci][:, h, :], KE[:, h, cs], QE[:, h, cs], start=(h == 0), stop=True,
                                 skip_group_check=True), reads=[("KE", sb_, h), ("QE", sb_, h)], writes=[("psc", ci)])
                p.op("dve", C("tensor_tensor", out=SCM[ci][:], in0=psc[ci][:], in1=cmask[:], op=ALU.mult),
                     reads=[("psc", ci), "cmask"], writes=[("SCM", ci)])
                for h in range(4):
                    p.op("pe", C("transpose", pkl[ci][:, h, :], KL[:, h, cs], k.ident[:]),
                         reads=[("KL", sb_, h), "ident"], writes=[("pkl", ci)])
                p.op("act", C("activation", out=KLT[ci][:], in_=pkl[ci][:], func=AF.Copy), reads=[("pkl", ci)], writes=[("KLT", ci)])
                for h in range(4):
                    hs = slice(h * 128, (h + 1) * 128)
                    p.op("pe", C("matmul", pkv[ci][:, h, :], KLT[ci][:, h, :], V[:, c, hs], start=(h == 0), stop=True,
                                 skip_group_check=True), reads=[("KLT", ci), ("V", sb_)], writes=[("pkv", ci)])

            def chunk_b(c):
                cs = slice(c * CH, (c + 1) * CH)
                ci = c % 2
                for h in range(4):
                    hs = slice(h * 128, (h + 1) * 128)
                    p.op("pe", C("matmul", pso[:, hs], SCM[ci][:, h, :], V[:, c, hs], start=(h == 0), stop=False,
                                 skip_group_check=True), reads=[("SCM", ci), ("V", sb_)], writes=["pso"])
                    p.op("pe", C("matmul", pso[:, hs], QE[:, h, cs], Sbf[:, h, :], start=False, stop=True,
                                 skip_group_check=True), reads=[("QE", sb_, h), ("Sbf", h)], writes=["pso"])
                p.op("act", C("activation", out=OS2[sb_][:, c, :], in_=pso[:], func=AF.Copy), reads=["pso"], writes=[("OS", sb_, c)])
                for h in range(4):
                    p.op("dve", C("scalar_tensor_tensor", out=S32[:, h, :], in0=S32[:, h, :],
                                  scalar=EB[:, h, c * CH + CH - 1:c * CH + CH], in1=pkv[ci][:, h, :], op0=ALU.mult, op1=ALU.add),
                         reads=[("S32", h), ("EB", sb_, h), ("pkv", ci)], writes=[("S32", h)])
                    p.op("act" if h % 2 else "dve", C("tensor_copy", Sbf[:, h, :], S32[:, h, :]) if not (h % 2) else
                         C("activation", out=Sbf[:, h, :], in_=S32[:, h, :], func=AF.Copy),
                         reads=[("S32", h)], writes=[("Sbf", h)])
            chunk_a(0)
            for c in range(NC_):
                if c + 1 < NC_:
                    chunk_a(c + 1)
                chunk_b(c)
                if seg + 1 < NSEG and c % 2 == 1:
                    ew_head(seg + 1, c // 2)
                if seg >= 1 and c % 2 == 0:
                    post_part(seg - 1, c // 2)
                if seg + 1 < NSEG and c == 5:
                    load_hog(seg + 1)
        for part in range(4):
            post_part(NSEG - 1, part)
        p.flush()


def pass_mixout(k, p, layer, xsrc):
    nc = k.nc
    S = k.S
    NSEG = S // 512
    with ExitStack() as st:
        def sb(name, shape, dt):
            return st.enter_context(nc.sbuf_tensor("%s_%d" % (name, p.nblk), list(shape), dt))

        def ps(name, shape, dt):
            return st.enter_context(nc.psum_tensor("%s_%d" % (name, p.nblk), list(shape), dt))
        PA = sb("PA", (128, 4, D), BF16)
        PB = sb("PB", (64, 8, D), BF16)
        WO = sb("WO", (128, 8, D), BF16)
        OA = sb("OA", (128, 4, 512), BF16)
        OB = sb("OB", (64, 8, 512), BF16)
        GA = [sb("GA%d" % i, (128, 512), F32) for i in range(2)]
        GB = [sb("GB%d" % i, (128, 512), F32) for i in range(2)]
        Y32 = [sb("Y32%d" % i, (128, 512), F32) for i in range(2)]
        Y32b = [sb("Y32b%d" % i, (128, 512), F32) for i in range(2)]
        YT = sb("YT", (128, 8, 512), BF16)
        X = sb("X", (128, 4, D), F32)
        XN = sb("XN", (128, 4, D), F32)
        ppa = [ps("ppa%d" % i, (128, 512), F32) for i in range(2)]
        ppb = [ps("ppb%d" % i, (128, 512), F32) for i in range(2)]
        ppo = [ps("ppo%d" % i, (128, 512), F32) for i in range(2)]
        kPA, kPB, kWO = [], [], []
        for hc in range(4):
            kPA += load_w_bf16(p, PA[:, hc, :], k.w_ba[layer, hc * 128:(hc + 1) * 128, :], ("PA", hc))
        for h in range(8):
            kPB += load_w_bf16(p, PB[:, h, :], k.w_bb[layer, h * 64:(h + 1) * 64, :], ("PB", h))
        for m in range(8):
            kWO += load_w_bf16(p, WO[:, m, :], k.w_out[layer, m * 128:(m + 1) * 128, :], ("WO", m))
        por = _ring(2)
        for seg in range(NSEG):
            tsl = slice(seg * 512, (seg + 1) * 512)
            p.dma("sp", OA[:], k.oaT[:, tsl].rearrange("(h p) t -> p h t", p=128), writes=["OA"])
            p.dma("sp", OB[:], k.obT[:, tsl].rearrange("(h d) t -> d h t", d=64), writes=["OB"])
            p.dma("sp", X[:], xsrc[tsl, :].rearrange("(j p) d -> p j d", p=128), writes=["X"])
            for m in range(8):
                i = m % 2
                ms = slice(m * 128, (m + 1) * 128)
                p.dma("sp", GA[i][:], k.gT[m * 128:(m + 1) * 128, tsl], writes=[("GA", i)])
                p.dma("sp", GB[i][:], k.gT[D + m * 128:D + (m + 1) * 128, tsl], writes=[("GB", i)])
                for hc in range(4):
                    p.op("pe", C("matmul", ppa[i][:], PA[:, hc, ms], OA[:, hc, :], start=(hc == 0), stop=(hc == 3)),
                         reads=kPA + ["OA"], writes=[("ppa", i)])
                for h in range(8):
                    p.op("pe", C("matmul", ppb[i][:], PB[:, h, ms], OB[:, h, :], start=(h == 0), stop=(h == 7)),
                         reads=kPB + ["OB"], writes=[("ppb", i)])
                p.op("dve", C("tensor_tensor", out=Y32[i][:], in0=ppa[i][:], in1=GA[i][:], op=ALU.mult),
                     reads=[("ppa", i), ("GA", i)], writes=[("Y32", i)])
                p.op("dve", C("tensor_tensor", out=Y32b[i][:], in0=ppb[i][:], in1=GB[i][:], op=ALU.mult),
                     reads=[("ppb", i), ("GB", i)], writes=[("Y32b", i)])
                p.op("pool", C("tensor_tensor", out=YT[:, m, :], in0=Y32[i][:], in1=Y32b[i][:], op=ALU.add),
                     reads=[("Y32", i), ("Y32b", i)], writes=[("YT", m)])
            ytk = [("YT", m) for m in range(8)]
            for j in range(4):
                for ch in range(2):
                    o_ = por()
                    for m in range(8):
                        p.op("pe", C("matmul", ppo[o_][:], YT[:, m, j * 128:(j + 1) * 128], WO[:, m, ch * 512:(ch + 1) * 512],
                                     start=(m == 0), stop=(m == 7)), reads=ytk + kWO, writes=[("ppo", o_)])
                    p.op("dve", C("tensor_tensor", out=XN[:, j, ch * 512:(ch + 1) * 512], in0=ppo[o_][:],
                                  in1=X[:, j, ch * 512:(ch + 1) * 512], op=ALU.add),
                         reads=[("ppo", o_), "X"], writes=[("XN", j, ch)])
            p.dma("sp", k.xres[tsl, :].rearrange("(j p) d -> p j d", p=128), XN[:],
                  reads=[("XN", j, ch) for j in range(4) for ch in range(2)], joins=["xres"], key=("o", "XN"))
        p.flush()


def norm_tile(p, xt_ap, xkey, h_ap, hkey, g, ss, rstd, j, sskey, extra_reads=()):
    p.op("dve", C("scalar_tensor_tensor", out=h_ap, in0=xt_ap, scalar=1.0, in1=xt_ap, op0=ALU.mult, op1=ALU.mult,
                  accum_out=ss[:, j:j + 1]), reads=[xkey] + list(extra_reads), writes=[hkey, (sskey, j)])
    p.op("dve", C("tensor_scalar", out=rstd[:, j:j + 1], in0=ss[:, j:j + 1], scalar1=1.0 / D, scalar2=EPS,
                  op0=ALU.mult, op1=ALU.add), reads=[(sskey, j)], writes=[(sskey, "r", j)])
    p.op("act", C("activation", out=rstd[:, j:j + 1], in_=rstd[:, j:j + 1], func=AF.Sqrt), reads=[(sskey, "r", j)], writes=[(sskey, "r", j)])
    p.op("dve", C("reciprocal", rstd[:, j:j + 1], rstd[:, j:j + 1]), reads=[(sskey, "r", j)], writes=[(sskey, "r", j)])
    p.op("dve", C("scalar_tensor_tensor", out=h_ap, in0=xt_ap, scalar=rstd[:, j:j + 1], in1=g[:], op0=ALU.mult, op1=ALU.mult),
         reads=[xkey, (sskey, "r", j), "g"], writes=[hkey])


def pass_router(k, p, layer):
    nc = k.nc
    S = k.S
    NSEG = S // 512
    jm = layer // 2
    with ExitStack() as st:
        def sb(name, shape, dt):
            return st.enter_context(nc.sbuf_tensor("%s_%d" % (name, p.nblk), list(shape), dt))

        def ps(name, shape, dt):
            return st.enter_context(nc.psum_tensor("%s_%d" % (name, p.nblk), list(shape), dt))
        g = sb("g", (128, D), F32)
        WR = sb("WR", (128, 8, NE), F32)
        X = [sb("X%d" % i, (128, 4, D), F32) for i in range(2)]
        HF = sb("HF", (128, 4, D), F32)
        HT = [sb("HT%d" % i, (128, 8, 128), F32) for i in range(2)]
        ss = sb("ss", (128, 4), F32)
        rstd = sb("rstd", (128, 4), F32)
        CB = [sb("CB%d" % i, (128, 4, NE), F32) for i in range(2)]
        sm = {n: sb("r_" + n, (128, NE), F32) for n in ("lg", "eq1", "lg2", "eq2")}
        sc = {n: sb("r_" + n, (128, 1), F32) for n in ("m1", "nm1", "m2", "ed", "w1", "w2")}
        ptr = [ps("ptr%d" % i, (128, 4, 128), F32) for i in range(4)]
        plg = [ps("plg%d" % i, (128, NE), F32) for i in range(2)]
        p.dma("sp", g[:], k.ffn_norm[layer, :].partition_broadcast(128), writes=["g"])
        p.dma("sp", WR[:], k.mrt[jm].rearrange("(c p) e -> p c e", p=128), writes=["WR"])
        ptr_r = _ring(4)
        p.dma("sp", X[0][:], k.xres[0:512, :].rearrange("(j p) d -> p j d", p=128), writes=[("X", 0)])
        for seg in range(NSEG):
            b = seg % 2
            if seg + 1 < NSEG:
                p.dma("sp", X[1 - b][:], k.xres[(seg + 1) * 512:(seg + 2) * 512, :].rearrange("(j p) d -> p j d", p=128),
                      writes=[("X", 1 - b)])
            for j in range(4):
                norm_tile(p, X[b][:, j, :], ("X", b), HF[:, j, :], ("HF", j), g, ss, rstd, j, "ss")
                hb = j % 2
                for half in range(2):
                    t_ = ptr_r()
                    for q4 in range(4):
                        kc = half * 4 + q4
                        p.op("pe", C("transpose", ptr[t_][:, q4, :], HF[:, j, kc * 128:(kc + 1) * 128], k.identf[:]),
                             reads=[("HF", j), "identf"], writes=[("ptr", t_)])
                    p.op("act" if half else "dve", C("tensor_copy", HT[hb][:, half * 4:half * 4 + 4, :], ptr[t_][:]) if not half else
                         C("activation", out=HT[hb][:, half * 4:half * 4 + 4, :], in_=ptr[t_][:], func=AF.Copy),
                         reads=[("ptr", t_)], writes=[("HT", hb, half)])
                lb_ = j % 2
                for kc in range(8):
                    p.op("pe", C("matmul", plg[lb_][:], HT[hb][:, kc, :], WR[:, kc, :], start=(kc == 0), stop=(kc == 7)),
                         reads=[("HT", hb, 0), ("HT", hb, 1), "WR"], writes=[("plg", lb_)])
                lg, eq1, lg2, eq2 = sm["lg"], sm["eq1"], sm["lg2"], sm["eq2"]
                m1, nm1, m2, ed, w1, w2 = (sc[n] for n in ("m1", "nm1", "m2", "ed", "w1", "w2"))
                cb = CB[b][:, j, :]
                p.op("dve", C("tensor_copy", lg[:], plg[lb_][:]), reads=[("plg", lb_)], writes=["lg"])
                p.op("dve", C("tensor_reduce", out=m1[:], in_=lg[:], axis=AX.X, op=ALU.max), reads=["lg"], writes=["m1"])
                p.op("dve", C("tensor_scalar", out=eq1[:], in0=lg[:], scalar1=m1[:, 0:1], scalar2=None, op0=ALU.is_equal),
                     reads=["lg", "m1"], writes=["eq1"])
                p.op("dve", C("scalar_tensor_tensor", out=lg2[:], in0=eq1[:], scalar=-1e30, in1=lg[:], op0=ALU.mult, op1=ALU.add),
                     reads=["eq1", "lg"], writes=["lg2"])
                p.op("dve", C("tensor_reduce", out=m2[:], in_=lg2[:], axis=AX.X, op=ALU.max), reads=["lg2"], writes=["m2"])
                p.op("dve", C("tensor_scalar", out=eq2[:], in0=lg2[:], scalar1=m2[:, 0:1], scalar2=None, op0=ALU.is_equal),
                     reads=["lg2", "m2"], writes=["eq2"])
                p.op("dve", C("tensor_scalar", out=nm1[:], in0=m1[:], scalar1=-1.0, scalar2=None, op0=ALU.mult),
                     reads=["m1"], writes=["nm1"])
                p.op("act", C("activation", out=ed[:], in_=m2[:], func=AF.Exp, bias=nm1[:, 0:1]), reads=["m2", "nm1"], writes=["ed"])
                p.op("dve", C("tensor_scalar", out=w1[:], in0=ed[:], scalar1=1.0, scalar2=None, op0=ALU.add), reads=["ed"], writes=["w1"])
                p.op("dve", C("reciprocal", w1[:], w1[:]), reads=["w1"], writes=["w1"])
                p.op("dve", C("tensor_tensor", out=w2[:], in0=ed[:], in1=w1[:], op=ALU.mult), reads=["ed", "w1"], writes=["w2"])
                p.op("dve", C("tensor_scalar", out=cb, in0=eq1[:], scalar1=w1[:, 0:1], scalar2=None, op0=ALU.mult),
                     reads=["eq1", "w1"], writes=[("CB", b, j)])
                p.op("dve", C("scalar_tensor_tensor", out=cb, in0=eq2[:], scalar=w2[:, 0:1], in1=cb, op0=ALU.mult, op1=ALU.add),
                     reads=["eq2", "w2", ("CB", b, j)], writes=[("CB", b, j)])
            p.dma("sp", k.comb_d[seg * 512:(seg + 1) * 512, :].rearrange("(j p) e -> p j e", p=128), CB[b][:],
                  reads=[("CB", b, j) for j in range(4)], joins=["comb_d"], key=("o", "CB", b))
        p.flush()


def pass_ffn(k, p, layer, last):
    nc = k.nc
    S = k.S
    TH = min(S, 2048)
    NH = S // TH
    NTT = TH // 128
    NTG = TH // 512
    moe = (layer % 2 == 1)
    jj = layer // 2
    NFB = FFN // 512
    with ExitStack() as st:
        def sb(name, shape, dt):
            return st.enter_context(nc.sbuf_tensor("%s_%d" % (name, p.nblk), list(shape), dt))

        def ps(name, shape, dt):
            return st.enter_context(nc.psum_tensor("%s_%d" % (name, p.nblk), list(shape), dt))
        g = sb("g", (128, D), F32)
        gF = sb("gF", (128, D), F32) if last else None
        X = sb("X", (128, NTT, D), F32)
        h = sb("h", (128, 4, D), BF16)
        H2T = sb("H2T", (128, KC, TH), BF16)
        ss = sb("ss", (128, 4), F32)
        rstd = sb("rstd", (128, 4), F32)
        Wg = [sb("Wg%d" % i, (128, KC, 512), BF16) for i in range(2)]
        Wu = [sb("Wu%d" % i, (128, KC, 512), BF16) for i in range(2)]
        Wd = [sb("Wd%d" % i, (128, 4, D), BF16) for i in range(2)]
        SIG = [sb("SIG%d" % i, (128, 512), F32) for i in range(2)]
        TT = [sb("TT%d" % i, (128, 512), F32) for i in range(2)]
        ACTT = [sb("ACTT%d" % i, (128, 4, 512), BF16) for i in range(2)]
        CB = sb("CB", (128, NTT, NE), F32) if moe else None
        pT = [ps("pT%d" % i, (128, D), BF16) for i in range(2)]
        pg = [ps("pg%d" % i, (128, 512), F32) for i in range(2)]
        pu = [ps("pu%d" % i, (128, 512), F32) for i in range(2)]
        pd = [ps("pd%d" % i, (128, 512), F32) for i in range(2)]
        pT_r, pg_r, pd_r, sg_r, at_r = _ring(2), _ring(2), _ring(2), _ring(2), _ring(2)
        p.dma("sp", g[:], k.ffn_norm[layer, :].partition_broadcast(128), writes=["g"])
        if last:
            p.dma("sp", gF[:], k.final_norm.partition_broadcast(128), writes=["gF"])
        wslot = [0]

        def load_weights(e, fb):
            i = wslot[0] % 2
            wslot[0] += 1
            fs = slice(fb * 512, (fb + 1) * 512)
            if moe:
                sg_, su_, sd_ = k.mwg[jj, e], k.mwu[jj, e], k.mwd[jj, e]
            else:
                sg_, su_, sd_ = k.dwg[jj], k.dwu[jj], k.dwd[jj]
            for kc in range(KC):
                kw1 = dict(writes=[("Wg", i)]) if kc == 0 else dict(joins=[("Wg", i)], key=("Wg", i))
                kw2 = dict(writes=[("Wu", i)]) if kc == 0 else dict(joins=[("Wu", i)], key=("Wu", i))
                p.dma("pool", Wg[i][:, kc, :], sg_[kc * 128:(kc + 1) * 128, fs], **kw1)
                p.dma("pool", Wu[i][:, kc, :], su_[kc * 128:(kc + 1) * 128, fs], **kw2)
            for fc in range(4):
                kw3 = dict(writes=[("Wd", i)]) if fc == 0 else dict(joins=[("Wd", i)], key=("Wd", i))
                p.dma("pool", Wd[i][:, fc, :], sd_[fb * 512 + fc * 128:fb * 512 + (fc + 1) * 128, :], **kw3)
            return i
        work = [(e, fb) for e in (range(NE) if moe else [0]) for fb in range(NFB)]
        for hf in range(NH):
            t0 = hf * TH
            for tg in range(NTG):
                p.dma("sp", X[:, tg * 4:(tg + 1) * 4, :], k.xres[t0 + tg * 512:t0 + (tg + 1) * 512, :].rearrange("(j p) d -> p j d", p=128),
                      writes=[("X", tg)])
            if moe:
                p.dma("sp", CB[:], k.comb_d[t0:t0 + TH, :].rearrange("(j p) e -> p j e", p=128), writes=["CB"])
            nxt = load_weights(*work[0])
            for tg in range(NTG):
                for j in range(4):
                    tt = tg * 4 + j
                    norm_tile(p, X[:, tt, :], ("X", tg), h[:, j, :], ("h", j), g, ss, rstd, j, "ss")
                    tb = pT_r()
                    for kc in range(KC):
                        p.op("pe", C("transpose", pT[tb][:, kc * 128:(kc + 1) * 128], h[:, j, kc * 128:(kc + 1) * 128], k.ident[:]),
                             reads=[("h", j), "ident"], writes=[("pT", tb)])
                    p.op("act", C("activation", out=H2T[:, :, tt * 128:(tt + 1) * 128],
                                  in_=pT[tb][:].rearrange("p (c t) -> p c t", c=KC), func=AF.Copy),
                         reads=[("pT", tb)], writes=[("H2T", tt)])
            units = [(wi_, tg) for wi_ in range(len(work)) for tg in range(NTG)]
            slots = {0: nxt}
            if len(work) > 1:
                slots[1] = load_weights(*work[1])
            ust = {}

            def gate_up(u):
                wi_, tg = units[u]
                wi = slots[wi_]
                wgk = [("Wg", wi)]
                wuk = [("Wu", wi)]
                hk = [("H2T", tg * 4 + j) for j in range(4)]
                ai = at_r()
                for fc in range(4):
                    gi = pg_r()
                    for kc in range(KC):
                        p.op("pe", C("matmul", pg[gi][:], Wg[wi][:, kc, fc * 128:(fc + 1) * 128], H2T[:, kc, tg * 512:(tg + 1) * 512],
                                     start=(kc == 0), stop=(kc == KC - 1)), reads=hk + wgk, writes=[("pg", gi)])
                    for kc in range(KC):
                        p.op("pe", C("matmul", pu[gi][:], Wu[wi][:, kc, fc * 128:(fc + 1) * 128], H2T[:, kc, tg * 512:(tg + 1) * 512],
                                     start=(kc == 0), stop=(kc == KC - 1)), reads=hk + wuk, writes=[("pu", gi)])
                    si = sg_r()
                    p.op("act", C("activation", out=SIG[si][:], in_=pg[gi][:], func=AF.Sigmoid), reads=[("pg", gi)], writes=[("SIG", si)])
                    p.op("dve", C("tensor_tensor", out=TT[si][:], in0=pg[gi][:], in1=SIG[si][:], op=ALU.mult),
                         reads=[("pg", gi), ("SIG", si)], writes=[("TT", si)])
                    p.op("dve", C("tensor_tensor", out=ACTT[ai][:, fc, :], in0=pu[gi][:], in1=TT[si][:], op=ALU.mult),
                         reads=[("pu", gi), ("TT", si)], writes=[("ACTT", ai, fc)])
                ust[u] = ai

            def down(u):
                wi_, tg = units[u]
                e, fb = work[wi_]
                wi = slots[wi_]
                wdk = [("Wd", wi)]
                ai = ust.pop(u)
                ak = [("ACTT", ai, fc) for fc in range(4)]
                for j in range(4):
                    tt = tg * 4 + j
                    for ch in range(2):
                        di = pd_r()
                        for fc in range(4):
                            p.op("pe", C("matmul", pd[di][:], ACTT[ai][:, fc, j * 128:(j + 1) * 128], Wd[wi][:, fc, ch * 512:(ch + 1) * 512],
                                         start=(fc == 0), stop=(fc == 3)), reads=ak + wdk, writes=[("pd", di)])
                        xs = X[:, tt, ch * 512:(ch + 1) * 512]
                        if moe:
                            p.op("dve", C("scalar_tensor_tensor", out=xs, in0=pd[di][:], scalar=CB[:, tt, e:e + 1], in1=xs,
                                          op0=ALU.mult, op1=ALU.add), reads=[("pd", di), "CB", ("X", tg)], writes=[("X", tg)])
                        else:
                            p.op("dve", C("tensor_tensor", out=xs, in0=pd[di][:], in1=xs, op=ALU.add),
                                 reads=[("pd", di), ("X", tg)], writes=[("X", tg)])
            nu = len(units)
            gate_up(0)
            for u in range(nu):
                if u + 1 < nu:
                    gate_up(u + 1)
                down(u)
                wi_, tg = units[u]
                if tg == NTG - 1 and wi_ + 2 < len(work):
                    slots[wi_ + 2] = load_weights(*work[wi_ + 2])
            for tg in range(NTG):
                if last:
                    for j in range(4):
                        tt = tg * 4 + j
                        p.op("dve", C("scalar_tensor_tensor", out=h[:, j, :], in0=X[:, tt, :], scalar=1.0, in1=X[:, tt, :],
                                      op0=ALU.mult, op1=ALU.mult, accum_out=ss[:, j:j + 1]), reads=[("X", tg)], writes=[("h", j), ("ssf", j)])
                        p.op("dve", C("tensor_scalar", out=rstd[:, j:j + 1], in0=ss[:, j:j + 1], scalar1=1.0 / D, scalar2=EPS,
                                      op0=ALU.mult, op1=ALU.add), reads=[("ssf", j)], writes=[("ssf", "r", j)])
                        p.op("act", C("activation", out=rstd[:, j:j + 1], in_=rstd[:, j:j + 1], func=AF.Sqrt),
                             reads=[("ssf", "r", j)], writes=[("ssf", "r", j)])
                        p.op("dve", C("reciprocal", rstd[:, j:j + 1], rstd[:, j:j + 1]), reads=[("ssf", "r", j)], writes=[("ssf", "r", j)])
                        p.op("dve", C("scalar_tensor_tensor", out=X[:, tt, :], in0=X[:, tt, :], scalar=rstd[:, j:j + 1], in1=gF[:],
                                      op0=ALU.mult, op1=ALU.mult), reads=[("X", tg), ("ssf", "r", j), "gF"], writes=[("X", tg)])
                dst = k.out if last else k.xres
                p.dma("sp", dst[t0 + tg * 512:t0 + (tg + 1) * 512, :].rearrange("(j p) d -> p j d", p=128), X[:, tg * 4:(tg + 1) * 4, :],
                      reads=[("X", tg)], joins=["xout"], key=("o", "X", tg))
        p.flush()


def kernel(**inputs):
    x = np.asarray(inputs["x"], dtype=np.float32)
    B, S, _ = x.shape
    nc = build(S=S, depth=4)
    names = ["mix_norm", "w_in", "hgrn_lb_logits", "hgrn_out_norm", "w_branch_hgrn", "w_branch_sb", "w_out", "ffn_norm",
             "dense_w_gate", "dense_w_up", "dense_w_down", "moe_router", "moe_w_gate", "moe_w_up", "moe_w_down", "final_norm"]
    shared = {n: np.ascontiguousarray(np.asarray(inputs[n], dtype=np.float32)) for n in names}
    in_maps = []
    for b in range(B):
        m = dict(shared)
        m["x"] = np.ascontiguousarray(x[b])
        in_maps.append(m)
    res = run_bass_kernel_spmd(nc, in_maps, core_ids=list(range(B)))
    return np.stack([np.asarray(r["out"], dtype=np.float32) for r in res.results], axis=0)
```

```python
import numpy as np
from contextlib import ExitStack
import concourse.bass as bass
import concourse.mybir as mybir
from concourse.bass_utils import run_bass_kernel_spmd

F32 = mybir.dt.float32
BF16 = mybir.dt.bfloat16
AF = mybir.ActivationFunctionType
ALU = mybir.AluOpType
AX = mybir.AxisListType

D = 1024
KC = 8
NIN = 5632
HW = 512
FFN = 3584
NE = 8
EPS = 1e-6
CH = 32
ENG = ("pe", "act", "dve", "pool", "sp")


class DSem:
    __slots__ = ("h", "count", "name")

    def __init__(self, name):
        self.h = None
        self.count = 0
        self.name = name


class Prog:
    def __init__(self, nc, st):
        self.nc = nc
        self.st = st
        self.ops = {e: [] for e in ENG}
        self.esem = {e: DSem("e_" + e) for e in ENG}
        for s in self.esem.values():
            s.h = st.enter_context(nc.semaphore(s.name))
        self.dsems = {}
        self.all_ds = []
        self.free_ds = []
        self.writers = {}
        self.readers = {}
        self.known = {e: {} for e in ENG}
        self.nblk = 0

    def _deps(self, eng, reads, writes):
        deps = {}

        def add(d):
            for s, v in d.items():
                if deps.get(s, 0) < v:
                    deps[s] = v
        for b in reads:
            add(self.writers.get(b, {}))
        for b in writes:
            add(self.writers.get(b, {}))
            add(self.readers.get(b, {}))
        waits = []
        kn = self.known[eng]
        for s, v in deps.items():
            if eng == "pe" and s is self.esem["pe"]:
                continue
            if kn.get(s, 0) >= v:
                continue
            kn[s] = v
            waits.append((s, v))
        return waits

    def _commit(self, reads, writes, s, v):
        for b in writes:
            self.writers[b] = {s: v}
            self.readers[b] = {}
        for b in reads:
            r = self.readers.setdefault(b, {})
            if r.get(s, 0) < v:
                r[s] = v

    def op(self, eng, call, reads=(), writes=()):
        m, a, kw = call

        def fn(e, m=m, a=a, kw=kw):
            return getattr(e, m)(*a, **kw)
        waits = self._deps(eng, reads, writes)
        s = self.esem[eng]
        s.count += 1
        self.ops[eng].append((waits, fn, (s, 1)))
        self._commit(reads, writes, s, s.count)

    def dma(self, q, out, in_, reads=(), writes=(), joins=(), key=None, **kw):
        if key is None:
            key = writes[0]
        ds = self.dsems.get(key)
        if ds is None:
            if self.free_ds:
                ds = self.free_ds.pop()
            else:
                ds = DSem("d%d" % len(self.all_ds))
                ds.h = self.st.enter_context(self.nc.semaphore(ds.name))
                self.all_ds.append(ds)
            self.dsems[key] = ds
        waits = self._deps(q, reads, writes)
        ds.count += 16

        def fn(e, out=out, in_=in_, kw=kw):
            return e.dma_start(out=out, in_=in_, **kw)
        self.ops[q].append((waits, fn, (ds, 16)))
        self._commit(reads, writes, ds, ds.count)
        for b in joins:
            w = self.writers.setdefault(b, {})
            w[ds] = ds.count

    def wait_all(self, eng, keys):
        waits = self._deps(eng, keys, ())
        self.ops[eng].append((waits, None, None))

    def flush(self):
        nc = self.nc
        allsems = list(self.esem.values()) + list(self.all_ds)
        for e in ENG:
            waits = []
            kn = self.known[e]
            for s in allsems:
                if s.count > 0 and kn.get(s, 0) < s.count:
                    kn[s] = s.count
                    waits.append((s, s.count))
            self.ops[e].append((waits, None, None))
        ops = self.ops
        self.ops = {e: [] for e in ENG}
        self.writers = {}
        self.readers = {}
        self.free_ds = list(self.all_ds)
        self.dsems = {}
        self.nblk += 1
        with nc.Block() as block:
            def mk(e):
                def body(engobj):
                    for waits, fn, inc in ops[e]:
                        for s, v in waits:
                            engobj.wait_ge(s.h, v)
                        if fn is not None:
                            fn(engobj).then_inc(inc[0].h, inc[1])
                return body
            block.tensor(mk("pe"))
            block.scalar(mk("act"))
            block.vector(mk("dve"))
            block.gpsimd(mk("pool"))
            block.sync(mk("sp"))


def C(m, *a, **kw):
    return (m, a, kw)


def _ring(n):
    i = [0]

    def nxt():
        v = i[0] % n
        i[0] += 1
        return v
    return nxt


class K:
    pass


def build(S=4096, depth=4, debug=False, stop_after=None):
    nc = bass.Bass("TRN2", target_bir_lowering=False)
    NSEG = S // 512
    NT = S // 128
    k = K()
    k.nc = nc
    k.S = S

    def din(name, shape):
        return nc.dram_tensor(name, list(shape), F32, kind="ExternalInput").ap()
    x_in = din("x", (S, D))
    mix_norm = din("mix_norm", (depth, D))
    w_in = din("w_in", (depth, D, NIN))
    lb_logits = din("hgrn_lb_logits", (4, HW))
    out_norm = din("hgrn_out_norm", (depth, 128))
    w_ba = din("w_branch_hgrn", (depth, HW, D))
    w_bb = din("w_branch_sb", (depth, HW, D))
    w_out = din("w_out", (depth, D, D))
    ffn_norm = din("ffn_norm", (depth, D))
    dwg = din("dense_w_gate", (2, D, FFN))
    dwu = din("dense_w_up", (2, D, FFN))
    dwd = din("dense_w_down", (2, FFN, D))
    mrt = din("moe_router", (2, D, NE))
    mwg = din("moe_w_gate", (2, NE, D, FFN))
    mwu = din("moe_w_up", (2, NE, D, FFN))
    mwd = din("moe_w_down", (2, NE, FFN, D))
    final_norm = din("final_norm", (D,))
    out = nc.dram_tensor("out", [S, D], F32, kind="ExternalOutput").ap()

    skind = "ExternalOutput" if debug else "Internal"

    def scr(name, shape, dt):
        return nc.dram_tensor(name, list(shape), dt, kind=skind).ap()
    xres = scr("xres", (S, D), F32)
    hqT = scr("hqT", (HW, S), F32)
    hfT = scr("hfT", (HW, S), F32)
    hi_d = scr("hi_d", (S, HW), BF16)
    hog_d = scr("hog_d", (S, HW), F32)
    sqT = scr("sqT", (HW, S), BF16)
    skT = scr("skT", (HW, S), BF16)
    sv_d = scr("sv_d", (S, HW), BF16)
    gT = scr("gT", (2 * D, S), F32)
    oaT = scr("oaT", (HW, S), BF16)
    obT = scr("obT", (HW, S), BF16)
    comb_d = scr("comb_d", (S, NE), F32)

    with ExitStack() as st:
        p = Prog(nc, st)
        ident = st.enter_context(nc.sbuf_tensor("ident", [128, 128], BF16))
        identf = st.enter_context(nc.sbuf_tensor("identf", [128, 128], F32))
        lbs = st.enter_context(nc.sbuf_tensor("lbs", [128, 4, 4], F32))
        ln1m = st.enter_context(nc.sbuf_tensor("ln1m", [128, 4, 4], F32))
        with ExitStack() as s0:
            lg16 = s0.enter_context(nc.sbuf_tensor("lg16", [16, 128], F32))
            ex = s0.enter_context(nc.sbuf_tensor("ex", [128, 4, 4], F32))
            sm = s0.enter_context(nc.sbuf_tensor("smx", [128, 4], F32))
            pl = s0.enter_context(nc.psum_tensor("pl", [128, 16], F32))
            p.op("pool", C("memset", identf[:], 1.0), writes=["identf"])
            p.op("pool", C("affine_select",
                out=identf[:], in_=identf[:], pattern=[[-1, 128]], compare_op=ALU.is_equal,
                fill=0.0, base=0, channel_multiplier=1), reads=["identf"], writes=["identf"])
            p.op("dve", C("tensor_copy", ident[:], identf[:]), reads=["identf"], writes=["ident"])
            p.dma("sp", lg16[:], lb_logits.rearrange("l (h p) -> (l h) p", p=128), writes=["lg16"])
            p.op("pe", C("transpose", pl[:], lg16[:], identf[0:16, 0:16]), reads=["lg16", "identf"], writes=["pl"])
            p.op("act", C("activation", out=ex[:].rearrange("p l h -> p (l h)"), in_=pl[:], func=AF.Exp),
                 reads=["pl"], writes=["ex"])
            p.op("dve", C("tensor_tensor", out=sm[:], in0=ex[:, 0, :], in1=ex[:, 1, :], op=ALU.add),
                 reads=["ex"], writes=["sm"])
            p.op("dve", C("tensor_tensor", out=sm[:], in0=sm[:], in1=ex[:, 2, :], op=ALU.add),
                 reads=["ex", "sm"], writes=["sm"])
            p.op("dve", C("tensor_tensor", out=sm[:], in0=sm[:], in1=ex[:, 3, :], op=ALU.add),
                 reads=["ex", "sm"], writes=["sm"])
            p.op("dve", C("reciprocal", sm[:], sm[:]), reads=["sm"], writes=["sm"])
            p.op("dve", C("memset", lbs[:, 0, :], 0.0), writes=["lbs0"])
            p.op("dve", C("tensor_tensor", out=lbs[:, 1, :], in0=ex[:, 1, :], in1=sm[:], op=ALU.mult),
                 reads=["ex", "sm"], writes=["lbs1"])
            p.op("dve", C("tensor_tensor", out=ex[:, 2, :], in0=ex[:, 2, :], in1=sm[:], op=ALU.mult),
                 reads=["ex", "sm"], writes=["ex"])
            p.op("dve", C("tensor_tensor", out=ex[:, 3, :], in0=ex[:, 3, :], in1=sm[:], op=ALU.mult),
                 reads=["ex", "sm"], writes=["ex"])
            p.op("dve", C("tensor_tensor", out=lbs[:, 2, :], in0=lbs[:, 1, :], in1=ex[:, 2, :], op=ALU.add),
                 reads=["ex", "lbs1"], writes=["lbs2"])
            p.op("dve", C("tensor_tensor", out=lbs[:, 3, :], in0=lbs[:, 2, :], in1=ex[:, 3, :], op=ALU.add),
                 reads=["ex", "lbs2"], writes=["lbs3"])
            p.op("act", C("activation", out=ln1m[:], in_=lbs[:], func=AF.Ln, scale=-1.0, bias=1.0),
                 reads=["lbs0", "lbs1", "lbs2", "lbs3"], writes=["ln1m"])
            p.flush()
        k.__dict__.update(locals())
        for layer in range(depth):
            xsrc = x_in if layer == 0 else xres
            pass_inproj(k, p, layer, xsrc)
            if stop_after == ("p1", layer):
                break
            if stop_after != ("p2", layer):
                pass_sb(k, p, layer)
            if stop_after == ("p3", layer):
                break
            pass_hgrn(k, p, layer)
            if stop_after == ("p2", layer):
                break
            pass_mixout(k, p, layer, xsrc)
            if layer % 2 == 1:
                pass_router(k, p, layer)
            pass_ffn(k, p, layer, last=(layer == depth - 1))
    return nc


def load_w_bf16(p, dst, src, key, rows=128):
    n = src.shape[-1]
    c0 = 0
    i = 0
    while c0 < n:
        c1 = min(n, c0 + 2048)
        p.dma("pool", dst[:, c0:c1], src[:, c0:c1], writes=[(key, i)])
        c0 = c1
        i += 1
    return [(key, j) for j in range(i)]


def pass_inproj(k, p, layer, xsrc):
    nc = k.nc
    S = k.S
    NSEG = S // 512
    with ExitStack() as st:
        def sb(name, shape, dt):
            return st.enter_context(nc.sbuf_tensor("%s_%d" % (name, p.nblk), list(shape), dt))

        def ps(name, shape, dt):
            return st.enter_context(nc.psum_tensor("%s_%d" % (name, p.nblk), list(shape), dt))
        W = sb("W", (128, KC, NIN), BF16)
        g = sb("g", (128, D), F32)
        xt = [sb("xt%d" % i, (128, 4, D), F32) for i in range(2)]
        junk = sb("junk", (128, D), F32)
        h = sb("h", (128, 4, D), BF16)
        hT = [sb("hT%d" % i, (128, KC, 512), BF16) for i in range(2)]
        ss = sb("ss", (128, 4), F32)
        rstd = sb("rstd", (128, 4), F32)
        sig = [sb("sig%d" % i, (128, 512), F32) for i in range(2)]
        stf = [sb("stf%d" % i, (128, 512), F32) for i in range(4)]
        stb = [sb("stb%d" % i, (128, 512), BF16) for i in range(4)]
        pT = [ps("pT%d" % i, (128, D), BF16) for i in range(2)]
        pm = [ps("pm%d" % i, (128, 512), F32) for i in range(4)]
        pT_r, pm_r, sig_r, stf_r, stb_r = _ring(2), _ring(4), _ring(2), _ring(4), _ring(4)

        wkeys = []
        for kc in range(KC):
            wkeys.append(load_w_bf16(p, W[:, kc, :], k.w_in[layer, kc * 128:(kc + 1) * 128, :], ("W", kc)))
        p.dma("sp", g[:], k.mix_norm[layer, :].partition_broadcast(128), writes=["g"])

        def load_x(seg):
            b = seg % 2
            p.dma("sp", xt[b][:], xsrc[seg * 512:(seg + 1) * 512, :].rearrange("(j p) d -> p j d", p=128),
                  writes=[("xt", b)])
        load_x(0)
        for seg in range(NSEG):
            b = seg % 2
            if seg + 1 < NSEG:
                load_x(seg + 1)
            for j in range(4):
                p.op("dve", C("scalar_tensor_tensor",
                    out=junk[:], in0=xt[b][:, j, :], scalar=1.0, in1=xt[b][:, j, :],
                    op0=ALU.mult, op1=ALU.mult, accum_out=ss[:, j:j + 1]),
                    reads=[("xt", b)], writes=["junk", ("ss", j)])
            sskeys = [("ss", j) for j in range(4)]
            p.op("dve", C("tensor_scalar", out=rstd[:], in0=ss[:], scalar1=1.0 / D, scalar2=EPS,
                                                   op0=ALU.mult, op1=ALU.add), reads=sskeys, writes=["rstd"])
            p.op("act", C("activation", out=rstd[:], in_=rstd[:], func=AF.Sqrt), reads=["rstd"], writes=["rstd"])
            p.op("dve", C("reciprocal", rstd[:], rstd[:]), reads=["rstd"], writes=["rstd"])
            for j in range(4):
                p.op("dve", C("scalar_tensor_tensor",
                    out=h[:, j, :], in0=xt[b][:, j, :], scalar=rstd[:, j:j + 1], in1=g[:],
                    op0=ALU.mult, op1=ALU.mult), reads=[("xt", b), "rstd", "g"], writes=[("h", j)])
            for j in range(4):
                tb = pT_r()
                for kc in range(KC):
                    p.op("pe", C("transpose",
                        pT[tb][:, kc * 128:(kc + 1) * 128], h[:, j, kc * 128:(kc + 1) * 128], k.ident[:]),
                        reads=[("h", j), "ident"], writes=[("pT", tb)])
                p.op("dve", C("tensor_copy",
                    hT[b][:, :, j * 128:(j + 1) * 128], pT[tb][:].rearrange("p (c t) -> p c t", c=KC)),
                    reads=[("pT", tb)], writes=[("hT", b, j)])
            hTk = [("hT", b, j) for j in range(4)]
            def fm_chunk(col0):
                pb = pm_r()
                for kc in range(KC):
                    wk = [kk for kk in wkeys[kc]]
                    p.op("pe", C("matmul",
                        pm[pb][:], W[:, kc, col0:col0 + 128], hT[b][:, kc, :], start=(kc == 0), stop=(kc == KC - 1)),
                        reads=hTk + wk, writes=[("pm", pb)])
                return pb
            tsl = slice(seg * 512, (seg + 1) * 512)
            for c in range(4):
                pb = fm_chunk(0 + c * 128)
                sb_ = sig_r()
                fb = stf_r()
                p.op("act", C("activation", out=sig[sb_][:], in_=pm[pb][:], func=AF.Sigmoid),
                     reads=[("pm", pb)], writes=[("sig", sb_)])
                p.op("dve", C("tensor_tensor", out=stf[fb][:], in0=pm[pb][:], in1=sig[sb_][:], op=ALU.mult),
                     reads=[("pm", pb), ("sig", sb_)], writes=[("stf", fb)])
                p.dma("sp", k.hqT[c * 128:(c + 1) * 128, tsl], stf[fb][:], reads=[("stf", fb)], joins=["hqT"], key=("o", "stf", fb))
            for c in range(4):
                pb = fm_chunk(512 + c * 128)
                fb = stf_r()
                p.op("act", C("activation", out=stf[fb][:], in_=pm[pb][:], func=AF.Copy),
                     reads=[("pm", pb)], writes=[("stf", fb)])
                p.dma("sp", k.hfT[c * 128:(c + 1) * 128, tsl], stf[fb][:], reads=[("stf", fb)], joins=["hfT"], key=("o", "stf", fb))
            for c in range(8):
                pb = fm_chunk(2048 + c * 128 if c < 4 else 2560 + (c - 4) * 128)
                bb = stb_r()
                sc = 0.125 if c < 4 else 1.0
                p.op("dve", C("tensor_scalar", out=stb[bb][:], in0=pm[pb][:], scalar1=sc, scalar2=None, op0=ALU.mult),
                     reads=[("pm", pb)], writes=[("stb", bb)])
                dst = k.sqT if c < 4 else k.skT
                cc = c % 4
                p.dma("sp", dst[cc * 128:(cc + 1) * 128, tsl], stb[bb][:], reads=[("stb", bb)],
                      joins=["sqT" if c < 4 else "skT"], key=("o", "stb", bb))
            for c in range(16):
                pb = fm_chunk(3584 + c * 128)
                fb = stf_r()
                p.op("act", C("activation", out=stf[fb][:], in_=pm[pb][:], func=AF.Sigmoid),
                     reads=[("pm", pb)], writes=[("stf", fb)])
                p.dma("sp", k.gT[c * 128:(c + 1) * 128, tsl], stf[fb][:], reads=[("stf", fb)], joins=["gT"], key=("o", "stf", fb))
            for j in range(4):
                rsl = slice(seg * 512 + j * 128, seg * 512 + (j + 1) * 128)
                for which, col0 in (("hi", 1024), ("hog", 1536), ("sv", 3072)):
                    pb = pm_r()
                    for kc in range(KC):
                        p.op("pe", C("matmul",
                            pm[pb][:], hT[b][:, kc, j * 128:(j + 1) * 128], W[:, kc, col0:col0 + 512],
                            start=(kc == 0), stop=(kc == KC - 1)),
                            reads=[("hT", b, j)] + wkeys[kc], writes=[("pm", pb)])
                    if which == "hog":
                        sb_ = sig_r()
                        fb = stf_r()
                        p.op("act", C("activation", out=sig[sb_][:], in_=pm[pb][:], func=AF.Sigmoid),
                             reads=[("pm", pb)], writes=[("sig", sb_)])
                        p.op("dve", C("tensor_tensor", out=stf[fb][:], in0=pm[pb][:], in1=sig[sb_][:], op=ALU.mult),
                             reads=[("pm", pb), ("sig", sb_)], writes=[("stf", fb)])
                        p.dma("sp", k.hog_d[rsl, :], stf[fb][:], reads=[("stf", fb)], joins=["hog_d"], key=("o", "stf", fb))
                    else:
                        bb = stb_r()
                        p.op("act", C("activation", out=stb[bb][:], in_=pm[pb][:], func=AF.Copy),
                             reads=[("pm", pb)], writes=[("stb", bb)])
                        dst = k.hi_d if which == "hi" else k.sv_d
                        p.dma("sp", dst[rsl, :], stb[bb][:], reads=[("stb", bb)],
                              joins=["hi_d" if which == "hi" else "sv_d"], key=("o", "stb", bb))
        p.flush()


def pass_sb(k, p, layer):
    nc = k.nc
    S = k.S
    NT = S // 128
    NG = NT // 4
    LA = 3
    with ExitStack() as st:
        def sb(name, shape, dt):
            return st.enter_context(nc.sbuf_tensor("%s_%d" % (name, p.nblk), list(shape), dt))

        def ps(name, shape, dt):
            return st.enter_context(nc.psum_tensor("%s_%d" % (name, p.nblk), list(shape), dt))
        kT = sb("kT", (128, 4, S), BF16)
        qT = [sb("qT%d" % i, (128, 8, 512), BF16) for i in range(2)]
        v = sb("v", (128, NT, 512), BF16)
        msk = sb("msk", (128, 4, 4, 128), F32)
        ntri = sb("ntri", (128, 128), BF16)
        nones = sb("nones", (128, 128), BF16)
        tmpf = sb("tmpf", (128, 128), F32)
        NE_, NSP, NW, NC16 = 4, 6, 4, 5
        E = [sb("E%d" % i, (128, 512), F32) for i in range(NE_)]
        SPf = [sb("SPf%d" % i, (128, 512), F32) for i in range(2)]
        Wf = [sb("Wf%d" % i, (128, 512), F32) for i in range(2)]
        SP = [sb("SP%d" % i, (128, 512), BF16) for i in range(NSP)]
        Wt = [sb("Wt%d" % i, (128, 512), BF16) for i in range(NW)]
        C32 = [sb("C32%d" % i, (128, 512), F32) for i in range(2)]
        C16 = [sb("C16%d" % i, (128, 512), BF16) for i in range(NC16)]
        obt = [sb("obt%d" % i, (128, 512), BF16) for i in range(2)]
        zA = [ps("zA%d" % i, (128, 512), F32) for i in range(3)]
        zB = [ps("zB%d" % i, (128, 512), F32) for i in range(2)]
        po = [ps("po%d" % i, (128, 512), F32) for i in range(2)]
        E_r, SP_r, W_r, C16_r, zA_r, zB_r, obt_r, spf_r, wf_r = (_ring(NE_), _ring(NSP), _ring(NW), _ring(NC16), _ring(3),
                                                                 _ring(2), _ring(2), _ring(2), _ring(2))
        p.op("pool", C("memset", msk[:], 1.0), writes=["msk"])
        for r in range(4):
            p.op("pool", C("affine_select", out=msk[:, r, :, :], in_=msk[:, r, :, :], pattern=[[128, 4], [1, 128]],
                           compare_op=ALU.is_gt, fill=0.0, base=-128 * r, channel_multiplier=-1),
                 reads=["msk"], writes=["msk"])
        p.op("pool", C("memset", tmpf[:], -1.0), writes=["tmpf"])
        p.op("dve", C("tensor_copy", nones[:], tmpf[:]), reads=["tmpf"], writes=["nones"])
        p.op("pool", C("affine_select", out=tmpf[:], in_=tmpf[:], pattern=[[-1, 128]],
                       compare_op=ALU.is_ge, fill=0.0, base=0, channel_multiplier=1), reads=["tmpf", "nones"], writes=["tmpf"])
        p.op("dve", C("tensor_copy", ntri[:], tmpf[:]), reads=["tmpf"], writes=["ntri"])
        for c in range(4):
            p.dma("sp", kT[:, c, :], k.skT[c * 128:(c + 1) * 128, :], writes=[("kT", c)])
        for i in range(2):
            p.op("pool", C("memset", qT[i][:], 0.0), writes=[("qT", i, 0), ("qT", i, 1)])
        for j0 in range(0, NT, 8):
            j1 = min(NT, j0 + 8)
            p.dma("sp", v[:, j0:j1, :], k.sv_d[j0 * 128:j1 * 128, :].rearrange("(j p) n -> p j n", p=128),
                  writes=[("v", j0 // 8)])
        steps = [(m, hd, kb) for m in range(NG) for hd in range(8) for kb in range(4 * m + 3, -1, -1)]
        stt = {}
        cst = {"c32": 0}

        def stage_a(i):
            m, hd, kb = steps[i]
            top = (kb == 4 * m + 3)
            r = kb - 4 * m
            qi = m % 2
            if top and hd == 0:
                qsrc = k.sqT[:, m * 512:(m + 1) * 512].rearrange("(c two d) t -> two d c t", two=2, d=64)
                for half in range(2):
                    p.dma("sp", qT[qi][half * 64:(half + 1) * 64, half::2, :], qsrc[half], writes=[("qT", qi, half)])
            a = zA_r()
            p.op("pe", C("matmul", zA[a][:], kT[:, hd // 2, kb * 128:(kb + 1) * 128], qT[qi][:, hd, :], start=True, stop=True),
                 reads=[("kT", hd // 2), ("qT", qi, hd % 2)], writes=[("zA", a)])
            ei = E_r()
            si = SP_r()
            p.op("act", C("activation", out=E[ei][:], in_=zA[a][:], func=AF.Exp), reads=[("zA", a)], writes=[("E", ei)])
            if r >= 0:
                fi = spf_r()
                p.op("act", C("activation", out=SPf[fi][:], in_=E[ei][:], func=AF.Ln, bias=1.0),
                     reads=[("E", ei)], writes=[("SPf", fi)])
                p.op("dve", C("tensor_tensor", out=SP[si][:], in0=SPf[fi][:],
                              in1=msk[:, r, :, :].rearrange("p q t -> p (q t)"), op=ALU.mult),
                     reads=[("SPf", fi), "msk"], writes=[("SP", si)])
            else:
                p.op("act", C("activation", out=SP[si][:], in_=E[ei][:], func=AF.Ln, bias=1.0),
                     reads=[("E", ei)], writes=[("SP", si)])
            c_use = None if top else cst["c16"]
            if kb > 0:
                ci = C16_r()
                if top:
                    cn = cst["c32"] = 1 - cst["c32"]
                    p.op("dve", C("tensor_copy", C32[cn][:], SP[si][:]), reads=[("SP", si)], writes=[("C32", cn)])
                    p.op("dve", C("tensor_copy", C16[ci][:], SP[si][:]), reads=[("SP", si)], writes=[("C16", ci)])
                else:
                    co = cst["c32"]
                    cn = cst["c32"] = 1 - co
                    p.op("dve", C("tensor_tensor", out=C16[ci][:], in0=C32[co][:], in1=SP[si][:], op=ALU.add),
                         reads=[("SP", si), ("C32", co)], writes=[("C16", ci)])
                    p.op("dve", C("tensor_tensor", out=C32[cn][:], in0=C32[co][:], in1=SP[si][:], op=ALU.add),
                         reads=[("SP", si), ("C32", co)], writes=[("C32", cn)])
                cst["c16"] = ci
            stt[i] = (si, c_use, qi)

        def stage_b(i):
            m, hd, kb = steps[i]
            top = (kb == 4 * m + 3)
            r = kb - 4 * m
            si, c_use, qi = stt.pop(i)
            pob = hd % 2
            bz = zB_r()
            p.op("pe", C("matmul", zB[bz][:], kT[:, hd // 2, kb * 128:(kb + 1) * 128], qT[qi][:, hd, :], start=True, stop=False,
                         skip_group_check=True), reads=[("kT", hd // 2), ("qT", qi, hd % 2)], writes=[("zB", bz)])
            p.op("pe", C("matmul", zB[bz][:], ntri[:], SP[si][:], start=False, stop=top, skip_group_check=True),
                 reads=["ntri", ("SP", si)], writes=[("zB", bz)])
            if not top:
                p.op("pe", C("matmul", zB[bz][:], nones[:], C16[c_use][:], start=False, stop=True, skip_group_check=True),
                     reads=["nones", ("C16", c_use)], writes=[("zB", bz)])
            wi = W_r()
            if r >= 0:
                fi = wf_r()
                p.op("act", C("activation", out=Wf[fi][:], in_=zB[bz][:], func=AF.Exp), reads=[("zB", bz)], writes=[("Wf", fi)])
                p.op("dve", C("tensor_tensor", out=Wt[wi][:], in0=Wf[fi][:],
                              in1=msk[:, r, :, :].rearrange("p q t -> p (q t)"), op=ALU.mult),
                     reads=[("Wf", fi), "msk"], writes=[("W", wi)])
            else:
                p.op("act", C("activation", out=Wt[wi][:], in_=zB[bz][:], func=AF.Exp), reads=[("zB", bz)], writes=[("W", wi)])
            stt[("b", i)] = wi

        def stage_b2(i):
            m, hd, kb = steps[i]
            top = (kb == 4 * m + 3)
            pob = hd % 2
            wi = stt.pop(("b", i))
            pc = (hd // 2) * 128
            p.op("pe", C("matmul", po[pob][:], v[:, kb, pc:pc + 128], Wt[wi][:], start=top, stop=(kb == 0),
                         skip_group_check=True), reads=[("v", kb // 8), ("W", wi)], writes=[("po", pob)])
            if kb == 0:
                oi = obt_r()
                hr = slice((hd % 2) * 64, (hd % 2) * 64 + 64)
                p.op("dve", C("tensor_copy", obt[oi][hr, :], po[pob][hr, :]), reads=[("po", pob)], writes=[("obt", oi)])
                p.dma("sp", k.obT[hd * 64:(hd + 1) * 64, m * 512:(m + 1) * 512], obt[oi][hr, :], reads=[("obt", oi)],
                      joins=["obT"], key=("o", "obt", oi))
        n = len(steps)
        for i in range(min(LA, n)):
            stage_a(i)
        for i in range(n):
            if i + LA < n:
                stage_a(i + LA)
            stage_b(i)
            if i >= 1:
                stage_b2(i - 1)
        stage_b2(n - 1)
        p.flush()


def pass_hgrn(k, p, layer):
    nc = k.nc
    S = k.S
    T = 256
    NC_ = T // CH
    NSEG = S // T
    with ExitStack() as st:
        def sb(name, shape, dt):
            return st.enter_context(nc.sbuf_tensor("%s_%d" % (name, p.nblk), list(shape), dt))

        def ps(name, shape, dt):
            return st.enter_context(nc.psum_tensor("%s_%d" % (name, p.nblk), list(shape), dt))
        ZF2 = [sb("ZF%d" % i, (128, 4, T), F32) for i in range(2)]
        Q2 = [sb("Q%d" % i, (128, 4, T), F32) for i in range(2)]
        V2 = [sb("V%d" % i, (32, NC_, 512), BF16) for i in range(2)]
        HOG2 = [sb("HOG%d" % i, (32, NC_, 512), F32) for i in range(2)]
        EB2 = [sb("EB%d" % i, (128, 4, T), F32) for i in range(2)]
        QE2 = [sb("QE%d" % i, (128, 4, T), BF16) for i in range(2)]
        KE2 = [sb("KE%d" % i, (128, 4, T), BF16) for i in range(2)]
        KL2 = [sb("KL%d" % i, (128, 4, T), BF16) for i in range(2)]
        tn = ("E", "L1", "L2", "LF", "Bc", "T1", "KK", "ENB", "KE32")
        tmp = {n: [sb("%s%d" % (n, i), (128, T), F32) for i in range(2)] for n in tn}
        rmask = sb("rmask", (128, T), F32)
        cmask = sb("cmask", (32, 4, 32), F32)
        GN = sb("GN", (32, 128), F32)
        S32 = sb("S32", (128, 4, 128), F32)
        Sbf = sb("Sbf", (128, 4, 128), BF16)
        SCM = [sb("SCM%d" % i, (32, 4, 32), BF16) for i in range(2)]
        KLT = [sb("KLT%d" % i, (32, 4, 128), BF16) for i in range(2)]
        OS2 = [sb("OS%d" % i, (32, NC_, 512), F32) for i in range(2)]
        SQ = sb("SQ", (32, NC_, 512), F32)
        OG = sb("OG", (32, NC_, 512), BF16)
        ssum = sb("ssum", (32, NC_ * 4), F32)
        OAT = sb("OAT", (128, 4, T), BF16)
        psc = [ps("psc%d" % i, (32, 4, 32), F32) for i in range(2)]
        pkl = [ps("pkl%d" % i, (32, 4, 128), BF16) for i in range(2)]
        pso = ps("pso", (32, 512), F32)
        pkv = [ps("pkv%d" % i, (128, 4, 128), F32) for i in range(2)]
        pot1 = ps("pot", (128, 4, T), BF16)
        pot = [pot1[:, 0:2, :], pot1[:, 2:4, :]]
        p.op("pool", C("memset", rmask[:], 1.0), writes=["rmask"])
        p.op("pool", C("memset", rmask[:].rearrange("p (c t) -> p c t", t=CH)[:, :, 0:1], 0.0), reads=["rmask"], writes=["rmask"])
        p.op("pool", C("memset", cmask[:], 1.0), writes=["cmask"])
        p.op("pool", C("affine_select", out=cmask[:], in_=cmask[:], pattern=[[0, 4], [1, 32]], compare_op=ALU.is_ge,
                       fill=0.0, base=0, channel_multiplier=-1), reads=["cmask"], writes=["cmask"])
        p.op("dve", C("memset", S32[:], 0.0), writes=[("S32", h) for h in range(4)])
        p.op("dve", C("memset", Sbf[:], 0.0), writes=[("Sbf", h) for h in range(4)])
        p.dma("sp", GN[:], k.out_norm[layer, :].partition_broadcast(32), writes=["GN"])
        lbv = k.lbs
        l1v = k.ln1m
        tr = _ring(2)
        def loads(seg):
            sb_ = seg % 2
            tsl = slice(seg * T, (seg + 1) * T)
            p.dma("sp", ZF2[sb_][:], k.hfT[:, tsl].rearrange("(h p) t -> p h t", p=128), writes=[("ZF", sb_)])
            p.dma("sp", Q2[sb_][:], k.hqT[:, tsl].rearrange("(h p) t -> p h t", p=128), writes=[("Q", sb_)])
            p.dma("sp", V2[sb_][:], k.hi_d[tsl, :].rearrange("(c p) n -> p c n", p=CH), writes=[("V", sb_)])
            if seg == 0:
                load_hog(seg)

        def load_hog(seg):
            sb_ = seg % 2
            tsl = slice(seg * T, (seg + 1) * T)
            p.dma("sp", HOG2[sb_][:], k.hog_d[tsl, :].rearrange("(c p) n -> p c n", p=CH), writes=[("HOG", sb_)])

        def ew_head(seg, h):
            sb_ = seg % 2
            ZF, Q, EB, QE, KE, KL = ZF2[sb_], Q2[sb_], EB2[sb_], QE2[sb_], KE2[sb_], KL2[sb_]
            i = tr()
            t_ = {n: tmp[n][i] for n in tn}
            K_ = {n: (n, i) for n in tn}
            lb_ap = lbv[:, layer, h:h + 1]
            l1m_ap = l1v[:, layer, h:h + 1]
            zk, qk_ = ("ZF", sb_), ("Q", sb_)
            ebk, qek, kek, klk = ("EB", sb_, h), ("QE", sb_, h), ("KE", sb_, h), ("KL", sb_, h)
            p.op("act", C("activation", out=t_["E"][:], in_=ZF[:, h, :], func=AF.Exp, scale=-1.0), reads=[zk], writes=[K_["E"]])
            p.op("act", C("activation", out=t_["L1"][:], in_=t_["E"][:], func=AF.Ln, bias=1.0), reads=[K_["E"]], writes=[K_["L1"]])
            p.op("act", C("activation", out=t_["L2"][:], in_=t_["E"][:], func=AF.Ln, scale=lb_ap, bias=1.0),
                 reads=[K_["E"]], writes=[K_["L2"]])
            p.op("dve", C("tensor_tensor", out=t_["LF"][:], in0=t_["L2"][:], in1=t_["L1"][:], op=ALU.subtract),
                 reads=[K_["L1"], K_["L2"]], writes=[K_["LF"]])
            p.op("dve", C("tensor_tensor_scan", out=t_["Bc"][:], data0=rmask[:], data1=t_["LF"][:], initial=0.0,
                          op0=ALU.mult, op1=ALU.add), reads=["rmask", K_["LF"]], writes=[K_["Bc"]])
            p.op("dve", C("tensor_tensor", out=t_["T1"][:], in0=ZF[:, h, :], in1=t_["L1"][:], op=ALU.add),
                 reads=[zk, K_["L1"]], writes=[K_["T1"]])
            p.op("act", C("activation", out=t_["KK"][:], in_=t_["T1"][:], func=AF.Exp, scale=-1.0, bias=l1m_ap),
                 reads=[K_["T1"]], writes=[K_["KK"]])
            p.op("act", C("activation", out=EB[:, h, :], in_=t_["Bc"][:], func=AF.Exp), reads=[K_["Bc"]], writes=[ebk])
            p.op("act", C("activation", out=t_["ENB"][:], in_=t_["Bc"][:], func=AF.Exp, scale=-1.0),
                 reads=[K_["Bc"]], writes=[K_["ENB"]])
            p.op("dve", C("tensor_tensor", out=QE[:, h, :], in0=Q[:, h, :], in1=EB[:, h, :], op=ALU.mult),
                 reads=[qk_, ebk], writes=[qek])
            p.op("dve", C("tensor_tensor", out=t_["KE32"][:], in0=t_["KK"][:], in1=t_["ENB"][:], op=ALU.mult),
                 reads=[K_["KK"], K_["ENB"]], writes=[K_["KE32"]])
            p.op("act", C("activation", out=KE[:, h, :], in_=t_["KE32"][:], func=AF.Copy), reads=[K_["KE32"]], writes=[kek])
            p.op("dve", C("tensor_tensor", out=KL[:, h, :].rearrange("p (c t) -> p c t", t=CH),
                          in0=t_["KE32"][:].rearrange("p (c t) -> p c t", t=CH),
                          in1=EB[:, h, :].rearrange("p (c t) -> p c t", t=CH)[:, :, CH - 1:CH].broadcast_to([128, NC_, CH]),
                          op=ALU.mult), reads=[K_["KE32"], ebk], writes=[klk])
        def post_part(seg, part):
            sb_ = seg % 2
            tsl = slice(seg * T, (seg + 1) * T)
            OS, HOG = OS2[sb_], HOG2[sb_]
            osk = [("OS", sb_, c) for c in range(NC_)]
            NG = NC_ * 4
            osv = OS[:].rearrange("p c (h v) -> p (c h) v", v=128)
            if part == 0:
                p.op("act", C("activation", out=SQ[:], in_=OS[:], func=AF.Square), reads=osk, writes=["SQ"])
                p.op("dve", C("tensor_reduce", out=ssum[:], in_=SQ[:].rearrange("p c (h v) -> p (c h) v", v=128), axis=AX.X, op=ALU.add),
                     reads=["SQ"], writes=["ssum"])
                p.op("act", C("activation", out=ssum[:], in_=ssum[:], func=AF.Ln, scale=1.0 / 128, bias=EPS), reads=["ssum"], writes=["ssum"])
                p.op("act", C("activation", out=ssum[:], in_=ssum[:], func=AF.Exp, scale=-0.5), reads=["ssum"], writes=["ssum"])
            elif part == 1:
                p.op("dve", C("tensor_tensor", out=osv, in0=osv, in1=ssum[:].unsqueeze(2).broadcast_to([32, NG, 128]), op=ALU.mult),
                     reads=osk + ["ssum"], writes=[("OSn", sb_)])
                hgv = HOG[:].rearrange("p c (h v) -> p (c h) v", v=128)
                p.op("dve", C("tensor_tensor", out=hgv, in0=hgv, in1=GN[:].unsqueeze(1).broadcast_to([32, NG, 128]), op=ALU.mult),
                     reads=[("HOG", sb_), "GN"], writes=[("HOG", sb_)])
            else:
                if part == 2:
                    p.op("dve", C("tensor_tensor", out=OG[:], in0=OS[:], in1=HOG[:], op=ALU.mult),
                         reads=[("OSn", sb_), ("HOG", sb_)], writes=["OG"])
                hp = part - 2
                for h2 in range(2):
                    h = hp * 2 + h2
                    for c in range(NC_):
                        p.op("pe", C("transpose", pot[hp][:, h2, c * CH:(c + 1) * CH], OG[:, c, h * 128:(h + 1) * 128],
                                     k.ident[0:32, 0:32]), reads=["OG", "ident"], writes=[("pot", 0)])
                p.op("dve", C("tensor_copy", OAT[:, hp * 2:hp * 2 + 2, :], pot[hp]), reads=[("pot", 0)], writes=[("OAT", hp)])
                if part == 3:
                    p.dma("sp", k.oaT[:, tsl].rearrange("(h p) t -> p h t", p=128), OAT[:], reads=[("OAT", 0), ("OAT", 1)],
                          joins=["oaT"], key=("o", "OAT"))
        loads(0)
        for h in range(4):
            ew_head(0, h)
        for seg in range(NSEG):
            sb_ = seg % 2
            tsl = slice(seg * T, (seg + 1) * T)
            ZF, Q, V, HOG = ZF2[sb_], Q2[sb_], V2[sb_], HOG2[sb_]
            EB, QE, KE, KL = EB2[sb_], QE2[sb_], KE2[sb_], KL2[sb_]
            if seg + 1 < NSEG:
                loads(seg + 1)
            def chunk_a(c):
                cs = slice(c * CH, (c + 1) * CH)
                ci = c % 2
                for h in range(4):
                    p.op("pe", C("matmul", psc[ci][:, h, :], KE[:, h, cs], QE[:, h, cs], start=(h == 0), stop=True,
                                 skip_group_check=True), reads=[("KE", sb_, h), ("QE", sb_, h)], writes=[("psc", ci)])
                p.op("dve", C("tensor_tensor", out=SCM[ci][:], in0=psc[ci][:], in1=cmask[:], op=ALU.mult),
                     reads=[("psc", ci), "cmask"], writes=[("SCM", ci)])
                for h in range(4):
                    p.op("pe", C("transpose", pkl[ci][:, h, :], KL[:, h, cs], k.ident[:]),
                         reads=[("KL", sb_, h), "ident"], writes=[("pkl", ci)])
                p.op("act", C("activation", out=KLT[ci][:], in_=pkl[ci][:], func=AF.Copy), reads=[("pkl", ci)], writes=[("KLT", ci)])
                for h in range(4):
                    hs = slice(h * 128, (h + 1) * 128)
                    p.op("pe", C("matmul", pkv[ci][:, h, :], KLT[ci][:, h, :], V[:, c, hs], start=(h == 0), stop=True,
                                 skip_group_check=True), reads=[("KLT", ci), ("V", sb_)], writes=[("pkv", ci)])

            def chunk_b(c):
                cs = slice(c * CH, (c + 1) * CH)
                ci = c % 2
                for h in range(4):
                    hs = slice(h * 128, (h + 1) * 128)
                    p.op("pe", C("matmul", pso[:, hs], SCM[ci][:, h, :], V[:, c, hs], start=(h == 0), stop=False,
                                 skip_group_check=True), reads=[("SCM", ci), ("V", sb_)], writes=["pso"])
                    p.op("pe", C("matmul", pso[:, hs], QE[:, h, cs], Sbf[:, h, :], start=False, stop=True,
                                 skip_group_check=True), reads=[("QE", sb_, h), ("Sbf", h)], writes=["pso"])
                p.op("act", C("activation", out=OS2[sb_][:, c, :], in_=pso[:], func=AF.Copy), reads=["pso"], writes=[("OS", sb_, c)])
                for h in range(4):
                    p.op("dve", C("scalar_tensor_tensor", out=S32[:, h, :], in0=S32[:, h, :],
                                  scalar=EB[:, h, c * CH + CH - 1:c * CH + CH], in1=pkv[ci][:, h, :], op0=ALU.mult, op1=ALU.add),
                         reads=[("S32", h), ("EB", sb_, h), ("pkv", ci)], writes=[("S32", h)])
                    p.op("act" if h % 2 else "dve", C("tensor_copy", Sbf[:, h, :], S32[:, h, :]) if not (h % 2) else
                         C("activation", out=Sbf[:, h, :], in_=S32[:, h, :], func=AF.Copy),
                         reads=[("S32", h)], writes=[("Sbf", h)])
            chunk_a(0)
            for c in range(NC_):
                if c + 1 < NC_:
                    chunk_a(c + 1)
                chunk_b(c)
                if seg + 1 < NSEG and c % 2 == 1:
                    ew_head(seg + 1, c // 2)
                if seg >= 1 and c % 2 == 0:
                    post_part(seg - 1, c // 2)
                if seg + 1 < NSEG and c == 5:
                    load_hog(seg + 1)
        for part in range(4):
            post_part(NSEG - 1, part)
        p.flush()


def pass_mixout(k, p, layer, xsrc):
    nc = k.nc
    S = k.S
    NSEG = S // 512
    with ExitStack() as st:
        def sb(name, shape, dt):
            return st.enter_context(nc.sbuf_tensor("%s_%d" % (name, p.nblk), list(shape), dt))

        def ps(name, shape, dt):
            return st.enter_context(nc.psum_tensor("%s_%d" % (name, p.nblk), list(shape), dt))
        PA = sb("PA", (128, 4, D), BF16)
        PB = sb("PB", (64, 8, D), BF16)
        WO = sb("WO", (128, 8, D), BF16)
        OA = sb("OA", (128, 4, 512), BF16)
        OB = sb("OB", (64, 8, 512), BF16)
        GA = [sb("GA%d" % i, (128, 512), F32) for i in range(2)]
        GB = [sb("GB%d" % i, (128, 512), F32) for i in range(2)]
        Y32 = [sb("Y32%d" % i, (128, 512), F32) for i in range(2)]
        Y32b = [sb("Y32b%d" % i, (128, 512), F32) for i in range(2)]
        YT = sb("YT", (128, 8, 512), BF16)
        X = sb("X", (128, 4, D), F32)
        XN = sb("XN", (128, 4, D), F32)
        ppa = [ps("ppa%d" % i, (128, 512), F32) for i in range(2)]
        ppb = [ps("ppb%d" % i, (128, 512), F32) for i in range(2)]
        ppo = [ps("ppo%d" % i, (128, 512), F32) for i in range(2)]
        kPA, kPB, kWO = [], [], []
        for hc in range(4):
            kPA += load_w_bf16(p, PA[:, hc, :], k.w_ba[layer, hc * 128:(hc + 1) * 128, :], ("PA", hc))
        for h in range(8):
            kPB += load_w_bf16(p, PB[:, h, :], k.w_bb[layer, h * 64:(h + 1) * 64, :], ("PB", h))
        for m in range(8):
            kWO += load_w_bf16(p, WO[:, m, :], k.w_out[layer, m * 128:(m + 1) * 128, :], ("WO", m))
        por = _ring(2)
        for seg in range(NSEG):
            tsl = slice(seg * 512, (seg + 1) * 512)
            p.dma("sp", OA[:], k.oaT[:, tsl].rearrange("(h p) t -> p h t", p=128), writes=["OA"])
            p.dma("sp", OB[:], k.obT[:, tsl].rearrange("(h d) t -> d h t", d=64), writes=["OB"])
            p.dma("sp", X[:], xsrc[tsl, :].rearrange("(j p) d -> p j d", p=128), writes=["X"])
            for m in range(8):
                i = m % 2
                ms = slice(m * 128, (m + 1) * 128)
                p.dma("sp", GA[i][:], k.gT[m * 128:(m + 1) * 128, tsl], writes=[("GA", i)])
                p.dma("sp", GB[i][:], k.gT[D + m * 128:D + (m + 1) * 128, tsl], writes=[("GB", i)])
                for hc in range(4):
                    p.op("pe", C("matmul", ppa[i][:], PA[:, hc, ms], OA[:, hc, :], start=(hc == 0), stop=(hc == 3)),
                         reads=kPA + ["OA"], writes=[("ppa", i)])
                for h in range(8):
                    p.op("pe", C("matmul", ppb[i][:], PB[:, h, ms], OB[:, h, :], start=(h == 0), stop=(h == 7)),
                         reads=kPB + ["OB"], writes=[("ppb", i)])
                p.op("dve", C("tensor_tensor", out=Y32[i][:], in0=ppa[i][:], in1=GA[i][:], op=ALU.mult),
                     reads=[("ppa", i), ("GA", i)], writes=[("Y32", i)])
                p.op("dve", C("tensor_tensor", out=Y32b[i][:], in0=ppb[i][:], in1=GB[i][:], op=ALU.mult),
                     reads=[("ppb", i), ("GB", i)], writes=[("Y32b", i)])
                p.op("pool", C("tensor_tensor", out=YT[:, m, :], in0=Y32[i][:], in1=Y32b[i][:], op=ALU.add),
                     reads=[("Y32", i), ("Y32b", i)], writes=[("YT", m)])
            ytk = [("YT", m) for m in range(8)]
            for j in range(4):
                for ch in range(2):
                    o_ = por()
                    for m in range(8):
                        p.op("pe", C("matmul", ppo[o_][:], YT[:, m, j * 128:(j + 1) * 128], WO[:, m, ch * 512:(ch + 1) * 512],
                                     start=(m == 0), stop=(m == 7)), reads=ytk + kWO, writes=[("ppo", o_)])
                    p.op("dve", C("tensor_tensor", out=XN[:, j, ch * 512:(ch + 1) * 512], in0=ppo[o_][:],
                                  in1=X[:, j, ch * 512:(ch + 1) * 512], op=ALU.add),
                         reads=[("ppo", o_), "X"], writes=[("XN", j, ch)])
            p.dma("sp", k.xres[tsl, :].rearrange("(j p) d -> p j d", p=128), XN[:],
                  reads=[("XN", j, ch) for j in range(4) for ch in range(2)], joins=["xres"], key=("o", "XN"))
        p.flush()


def norm_tile(p, xt_ap, xkey, h_ap, hkey, g, ss, rstd, j, sskey, extra_reads=()):
    p.op("dve", C("scalar_tensor_tensor", out=h_ap, in0=xt_ap, scalar=1.0, in1=xt_ap, op0=ALU.mult, op1=ALU.mult,
                  accum_out=ss[:, j:j + 1]), reads=[xkey] + list(extra_reads), writes=[hkey, (sskey, j)])
    p.op("dve", C("tensor_scalar", out=rstd[:, j:j + 1], in0=ss[:, j:j + 1], scalar1=1.0 / D, scalar2=EPS,
                  op0=ALU.mult, op1=ALU.add), reads=[(sskey, j)], writes=[(sskey, "r", j)])
    p.op("act", C("activation", out=rstd[:, j:j + 1], in_=rstd[:, j:j + 1], func=AF.Sqrt), reads=[(sskey, "r", j)], writes=[(sskey, "r", j)])
    p.op("dve", C("reciprocal", rstd[:, j:j + 1], rstd[:, j:j + 1]), reads=[(sskey, "r", j)], writes=[(sskey, "r", j)])
    p.op("dve", C("scalar_tensor_tensor", out=h_ap, in0=xt_ap, scalar=rstd[:, j:j + 1], in1=g[:], op0=ALU.mult, op1=ALU.mult),
         reads=[xkey, (sskey, "r", j), "g"], writes=[hkey])


def pass_router(k, p, layer):
    nc = k.nc
    S = k.S
    NSEG = S // 512
    jm = layer // 2
    with ExitStack() as st:
        def sb(name, shape, dt):
            return st.enter_context(nc.sbuf_tensor("%s_%d" % (name, p.nblk), list(shape), dt))

        def ps(name, shape, dt):
            return st.enter_context(nc.psum_tensor("%s_%d" % (name, p.nblk), list(shape), dt))
        g = sb("g", (128, D), F32)
        WR = sb("WR", (128, 8, NE), F32)
        X = [sb("X%d" % i, (128, 4, D), F32) for i in range(2)]
        HF = sb("HF", (128, 4, D), F32)
        HT = [sb("HT%d" % i, (128, 8, 128), F32) for i in range(2)]
        ss = sb("ss", (128, 4), F32)
        rstd = sb("rstd", (128, 4), F32)
        CB = [sb("CB%d" % i, (128, 4, NE), F32) for i in range(2)]
        sm = {n: sb("r_" + n, (128, NE), F32) for n in ("lg", "eq1", "lg2", "eq2")}
        sc = {n: sb("r_" + n, (128, 1), F32) for n in ("m1", "nm1", "m2", "ed", "w1", "w2")}
        ptr = [ps("ptr%d" % i, (128, 4, 128), F32) for i in range(4)]
        plg = [ps("plg%d" % i, (128, NE), F32) for i in range(2)]
        p.dma("sp", g[:], k.ffn_norm[layer, :].partition_broadcast(128), writes=["g"])
        p.dma("sp", WR[:], k.mrt[jm].rearrange("(c p) e -> p c e", p=128), writes=["WR"])
        ptr_r = _ring(4)
        p.dma("sp", X[0][:], k.xres[0:512, :].rearrange("(j p) d -> p j d", p=128), writes=[("X", 0)])
        for seg in range(NSEG):
            b = seg % 2
            if seg + 1 < NSEG:
                p.dma("sp", X[1 - b][:], k.xres[(seg + 1) * 512:(seg + 2) * 512, :].rearrange("(j p) d -> p j d", p=128),
                      writes=[("X", 1 - b)])
            for j in range(4):
                norm_tile(p, X[b][:, j, :], ("X", b), HF[:, j, :], ("HF", j), g, ss, rstd, j, "ss")
                hb = j % 2
                for half in range(2):
                    t_ = ptr_r()
                    for q4 in range(4):
                        kc = half * 4 + q4
                        p.op("pe", C("transpose", ptr[t_][:, q4, :], HF[:, j, kc * 128:(kc + 1) * 128], k.identf[:]),
                             reads=[("HF", j), "identf"], writes=[("ptr", t_)])
                    p.op("act" if half else "dve", C("tensor_copy", HT[hb][:, half * 4:half * 4 + 4, :], ptr[t_][:]) if not half else
                         C("activation", out=HT[hb][:, half * 4:half * 4 + 4, :], in_=ptr[t_][:], func=AF.Copy),
                         reads=[("ptr", t_)], writes=[("HT", hb, half)])
                lb_ = j % 2
                for kc in range(8):
                    p.op("pe", C("matmul", plg[lb_][:], HT[hb][:, kc, :], WR[:, kc, :], start=(kc == 0), stop=(kc == 7)),
                         reads=[("HT", hb, 0), ("HT", hb, 1), "WR"], writes=[("plg", lb_)])
                lg, eq1, lg2, eq2 = sm["lg"], sm["eq1"], sm["lg2"], sm["eq2"]
                m1, nm1, m2, ed, w1, w2 = (sc[n] for n in ("m1", "nm1", "m2", "ed", "w1", "w2"))
                cb = CB[b][:, j, :]
                p.op("dve", C("tensor_copy", lg[:], plg[lb_][:]), reads=[("plg", lb_)], writes=["lg"])
                p.op("dve", C("tensor_reduce", out=m1[:], in_=lg[:], axis=AX.X, op=ALU.max), reads=["lg"], writes=["m1"])
                p.op("dve", C("tensor_scalar", out=eq1[:], in0=lg[:], scalar1=m1[:, 0:1], scalar2=None, op0=ALU.is_equal),
                     reads=["lg", "m1"], writes=["eq1"])
                p.op("dve", C("scalar_tensor_tensor", out=lg2[:], in0=eq1[:], scalar=-1e30, in1=lg[:], op0=ALU.mult, op1=ALU.add),
                     reads=["eq1", "lg"], writes=["lg2"])
                p.op("dve", C("tensor_reduce", out=m2[:], in_=lg2[:], axis=AX.X, op=ALU.max), reads=["lg2"], writes=["m2"])
                p.op("dve", C("tensor_scalar", out=eq2[:], in0=lg2[:], scalar1=m2[:, 0:1], scalar2=None, op0=ALU.is_equal),
                     reads=["lg2", "m2"], writes=["eq2"])
                p.op("dve", C("tensor_scalar", out=nm1[:], in0=m1[:], scalar1=-1.0, scalar2=None, op0=ALU.mult),
                     reads=["m1"], writes=["nm1"])
                p.op("act", C("activation", out=ed[:], in_=m2[:], func=AF.Exp, bias=nm1[:, 0:1]), reads=["m2", "nm1"], writes=["ed"])
                p.op("dve", C("tensor_scalar", out=w1[:], in0=ed[:], scalar1=1.0, scalar2=None, op0=ALU.add), reads=["ed"], writes=["w1"])
                p.op("dve", C("reciprocal", w1[:], w1[:]), reads=["w1"], writes=["w1"])
                p.op("dve", C("tensor_tensor", out=w2[:], in0=ed[:], in1=w1[:], op=ALU.mult), reads=["ed", "w1"], writes=["w2"])
                p.op("dve", C("tensor_scalar", out=cb, in0=eq1[:], scalar1=w1[:, 0:1], scalar2=None, op0=ALU.mult),
                     reads=["eq1", "w1"], writes=[("CB", b, j)])
                p.op("dve", C("scalar_tensor_tensor", out=cb, in0=eq2[:], scalar=w2[:, 0:1], in1=cb, op0=ALU.mult, op1=ALU.add),
                     reads=["eq2", "w2", ("CB", b, j)], writes=[("CB", b, j)])
            p.dma("sp", k.comb_d[seg * 512:(seg + 1) * 512, :].rearrange("(j p) e -> p j e", p=128), CB[b][:],
                  reads=[("CB", b, j) for j in range(4)], joins=["comb_d"], key=("o", "CB", b))
        p.flush()


def pass_ffn(k, p, layer, last):
    nc = k.nc
    S = k.S
    TH = min(S, 2048)
    NH = S // TH
    NTT = TH // 128
    NTG = TH // 512
    moe = (layer % 2 == 1)
    jj = layer // 2
    NFB = FFN // 512
    with ExitStack() as st:
        def sb(name, shape, dt):
            return st.enter_context(nc.sbuf_tensor("%s_%d" % (name, p.nblk), list(shape), dt))

        def ps(name, shape, dt):
            return st.enter_context(nc.psum_tensor("%s_%d" % (name, p.nblk), list(shape), dt))
        g = sb("g", (128, D), F32)
        gF = sb("gF", (128, D), F32) if last else None
        X = sb("X", (128, NTT, D), F32)
        h = sb("h", (128, 4, D), BF16)
        H2T = sb("H2T", (128, KC, TH), BF16)
        ss = sb("ss", (128, 4), F32)
        rstd = sb("rstd", (128, 4), F32)
        Wg = [sb("Wg%d" % i, (128, KC, 512), BF16) for i in range(2)]
        Wu = [sb("Wu%d" % i, (128, KC, 512), BF16) for i in range(2)]
        Wd = [sb("Wd%d" % i, (128, 4, D), BF16) for i in range(2)]
        SIG = [sb("SIG%d" % i, (128, 512), F32) for i in range(2)]
        TT = [sb("TT%d" % i, (128, 512), F32) for i in range(2)]
        ACTT = [sb("ACTT%d" % i, (128, 4, 512), BF16) for i in range(2)]
        CB = sb("CB", (128, NTT, NE), F32) if moe else None
        pT = [ps("pT%d" % i, (128, D), BF16) for i in range(2)]
        pg = [ps("pg%d" % i, (128, 512), F32) for i in range(2)]
        pu = [ps("pu%d" % i, (128, 512), F32) for i in range(2)]
        pd = [ps("pd%d" % i, (128, 512), F32) for i in range(2)]
        pT_r, pg_r, pd_r, sg_r, at_r = _ring(2), _ring(2), _ring(2), _ring(2), _ring(2)
        p.dma("sp", g[:], k.ffn_norm[layer, :].partition_broadcast(128), writes=["g"])
        if last:
            p.dma("sp", gF[:], k.final_norm.partition_broadcast(128), writes=["gF"])
        wslot = [0]

        def load_weights(e, fb):
            i = wslot[0] % 2
            wslot[0] += 1
            fs = slice(fb * 512, (fb + 1) * 512)
            if moe:
                sg_, su_, sd_ = k.mwg[jj, e], k.mwu[jj, e], k.mwd[jj, e]
            else:
                sg_, su_, sd_ = k.dwg[jj], k.dwu[jj], k.dwd[jj]
            for kc in range(KC):
                kw1 = dict(writes=[("Wg", i)]) if kc == 0 else dict(joins=[("Wg", i)], key=("Wg", i))
                kw2 = dict(writes=[("Wu", i)]) if kc == 0 else dict(joins=[("Wu", i)], key=("Wu", i))
                p.dma("pool", Wg[i][:, kc, :], sg_[kc * 128:(kc + 1) * 128, fs], **kw1)
                p.dma("pool", Wu[i][:, kc, :], su_[kc * 128:(kc + 1) * 128, fs], **kw2)
            for fc in range(4):
                kw3 = dict(writes=[("Wd", i)]) if fc == 0 else dict(joins=[("Wd", i)], key=("Wd", i))
                p.dma("pool", Wd[i][:, fc, :], sd_[fb * 512 + fc * 128:fb * 512 + (fc + 1) * 128, :], **kw3)
            return i
        work = [(e, fb) for e in (range(NE) if moe else [0]) for fb in range(NFB)]
        for hf in range(NH):
            t0 = hf * TH
            for tg in range(NTG):
                p.dma("sp", X[:, tg * 4:(tg + 1) * 4, :], k.xres[t0 + tg * 512:t0 + (tg + 1) * 512, :].rearrange("(j p) d -> p j d", p=128),
                      writes=[("X", tg)])
            if moe:
                p.dma("sp", CB[:], k.comb_d[t0:t0 + TH, :].rearrange("(j p) e -> p j e", p=128), writes=["CB"])
            nxt = load_weights(*work[0])
            for tg in range(NTG):
                for j in range(4):
                    tt = tg * 4 + j
                    norm_tile(p, X[:, tt, :], ("X", tg), h[:, j, :], ("h", j), g, ss, rstd, j, "ss")
                    tb = pT_r()
                    for kc in range(KC):
                        p.op("pe", C("transpose", pT[tb][:, kc * 128:(kc + 1) * 128], h[:, j, kc * 128:(kc + 1) * 128], k.ident[:]),
                             reads=[("h", j), "ident"], writes=[("pT", tb)])
                    p.op("act", C("activation", out=H2T[:, :, tt * 128:(tt + 1) * 128],
                                  in_=pT[tb][:].rearrange("p (c t) -> p c t", c=KC), func=AF.Copy),
                         reads=[("pT", tb)], writes=[("H2T", tt)])
            units = [(wi_, tg) for wi_ in range(len(work)) for tg in range(NTG)]
            slots = {0: nxt}
            if len(work) > 1:
                slots[1] = load_weights(*work[1])
            ust = {}

            def gate_up(u):
                wi_, tg = units[u]
                wi = slots[wi_]
                wgk = [("Wg", wi)]
                wuk = [("Wu", wi)]
                hk = [("H2T", tg * 4 + j) for j in range(4)]
                ai = at_r()
                for fc in range(4):
                    gi = pg_r()
                    for kc in range(KC):
                        p.op("pe", C("matmul", pg[gi][:], Wg[wi][:, kc, fc * 128:(fc + 1) * 128], H2T[:, kc, tg * 512:(tg + 1) * 512],
                                     start=(kc == 0), stop=(kc == KC - 1)), reads=hk + wgk, writes=[("pg", gi)])
                    for kc in range(KC):
                        p.op("pe", C("matmul", pu[gi][:], Wu[wi][:, kc, fc * 128:(fc + 1) * 128], H2T[:, kc, tg * 512:(tg + 1) * 512],
                                     start=(kc == 0), stop=(kc == KC - 1)), reads=hk + wuk, writes=[("pu", gi)])
                    si = sg_r()
                    p.op("act", C("activation", out=SIG[si][:], in_=pg[gi][:], func=AF.Sigmoid), reads=[("pg", gi)], writes=[("SIG", si)])
                    p.op("dve", C("tensor_tensor", out=TT[si][:], in0=pg[gi][:], in1=SIG[si][:], op=ALU.mult),
                         reads=[("pg", gi), ("SIG", si)], writes=[("TT", si)])
                    p.op("dve", C("tensor_tensor", out=ACTT[ai][:, fc, :], in0=pu[gi][:], in1=TT[si][:], op=ALU.mult),
                         reads=[("pu", gi), ("TT", si)], writes=[("ACTT", ai, fc)])
                ust[u] = ai

            def down(u):
                wi_, tg = units[u]
                e, fb = work[wi_]
                wi = slots[wi_]
                wdk = [("Wd", wi)]
                ai = ust.pop(u)
                ak = [("ACTT", ai, fc) for fc in range(4)]
                for j in range(4):
                    tt = tg * 4 + j
                    for ch in range(2):
                        di = pd_r()
                        for fc in range(4):
                            p.op("pe", C("matmul", pd[di][:], ACTT[ai][:, fc, j * 128:(j + 1) * 128], Wd[wi][:, fc, ch * 512:(ch + 1) * 512],
                                         start=(fc == 0), stop=(fc == 3)), reads=ak + wdk, writes=[("pd", di)])
                        xs = X[:, tt, ch * 512:(ch + 1) * 512]
                        if moe:
                            p.op("dve", C("scalar_tensor_tensor", out=xs, in0=pd[di][:], scalar=CB[:, tt, e:e + 1], in1=xs,
                                          op0=ALU.mult, op1=ALU.add), reads=[("pd", di), "CB", ("X", tg)], writes=[("X", tg)])
                        else:
                            p.op("dve", C("tensor_tensor", out=xs, in0=pd[di][:], in1=xs, op=ALU.add),
                                 reads=[("pd", di), ("X", tg)], writes=[("X", tg)])
            nu = len(units)
            if not moe:
                gate_up(0)
            for u in range(nu):
                if moe:
                    gate_up(u)
                elif u + 1 < nu:
                    gate_up(u + 1)
                down(u)
                wi_, tg = units[u]
                if tg == NTG - 1 and wi_ + 2 < len(work):
                    slots[wi_ + 2] = load_weights(*work[wi_ + 2])
            for tg in range(NTG):
                if last:
                    for j in range(4):
                        tt = tg * 4 + j
                        p.op("dve", C("scalar_tensor_tensor", out=h[:, j, :], in0=X[:, tt, :], scalar=1.0, in1=X[:, tt, :],
                                      op0=ALU.mult, op1=ALU.mult, accum_out=ss[:, j:j + 1]), reads=[("X", tg)], writes=[("h", j), ("ssf", j)])
                        p.op("dve", C("tensor_scalar", out=rstd[:, j:j + 1], in0=ss[:, j:j + 1], scalar1=1.0 / D, scalar2=EPS,
                                      op0=ALU.mult, op1=ALU.add), reads=[("ssf", j)], writes=[("ssf", "r", j)])
                        p.op("act", C("activation", out=rstd[:, j:j + 1], in_=rstd[:, j:j + 1], func=AF.Sqrt),
                             reads=[("ssf", "r", j)], writes=[("ssf", "r", j)])
                        p.op("dve", C("reciprocal", rstd[:, j:j + 1], rstd[:, j:j + 1]), reads=[("ssf", "r", j)], writes=[("ssf", "r", j)])
                        p.op("dve", C("scalar_tensor_tensor", out=X[:, tt, :], in0=X[:, tt, :], scalar=rstd[:, j:j + 1], in1=gF[:],
                                      op0=ALU.mult, op1=ALU.mult), reads=[("X", tg), ("ssf", "r", j), "gF"], writes=[("X", tg)])
                dst = k.out if last else k.xres
                p.dma("sp", dst[t0 + tg * 512:t0 + (tg + 1) * 512, :].rearrange("(j p) d -> p j d", p=128), X[:, tg * 4:(tg + 1) * 4, :],
                      reads=[("X", tg)], joins=["xout"], key=("o", "X", tg))
        p.flush()


def kernel(**inputs):
    x = np.asarray(inputs["x"], dtype=np.float32)
    B, S, _ = x.shape
    nc = build(S=S, depth=4)
    names = ["mix_norm", "w_in", "hgrn_lb_logits", "hgrn_out_norm", "w_branch_hgrn", "w_branch_sb", "w_out", "ffn_norm",
             "dense_w_gate", "dense_w_up", "dense_w_down", "moe_router", "moe_w_gate", "moe_w_up", "moe_w_down", "final_norm"]
    shared = {n: np.ascontiguousarray(np.asarray(inputs[n], dtype=np.float32)) for n in names}
    in_maps = []
    for b in range(B):
        m = dict(shared)
        m["x"] = np.ascontiguousarray(x[b])
        in_maps.append(m)
    res = run_bass_kernel_spmd(nc, in_maps, core_ids=list(range(B)))
    return np.stack([np.asarray(r["out"], dtype=np.float32) for r in res.results], axis=0)
```
